# Optimizing a Trainium2 kernel written in Bass

```python
import math
import numpy as np
import jax
import jax.numpy as jnp
from jax import lax

D_MODEL = 1024
BATCH = 8
SEQ = 4096
DEPTH = 4

CHUNK = 64
BRANCH_WIDTH = D_MODEL // 2
N_BRANCH = 3
M_HEADS = 4
M_HEAD_DIM = BRANCH_WIDTH // M_HEADS
M_CONV = 4
S5_GROUP = 16
S5_GROUPS = BRANCH_WIDTH // S5_GROUP
S5_STATE = 64
A_HEADS = 8
A_HEAD_DIM = BRANCH_WIDTH // A_HEADS
Q_BLOCK = 128
N_GROUPS = 4
EXPERTS_PER_GROUP = 8
N_EXPERTS = N_GROUPS * EXPERTS_PER_GROUP
EXPERT_HIDDEN = D_MODEL // 2
TOP_K = 2
MOE_BLOCK = 256
LN_EPS = 1e-5
ALPHA = (2 * DEPTH) ** 0.25
BETA = (8 * DEPTH) ** -0.25
IN_SIZES = (2 * BRANCH_WIDTH, BRANCH_WIDTH, BRANCH_WIDTH, M_HEADS, M_HEADS,
            BRANCH_WIDTH, 3 * BRANCH_WIDTH, A_HEADS, N_BRANCH * D_MODEL)
IN_TOTAL = 8 * BRANCH_WIDTH + 2 * M_HEADS + A_HEADS + N_BRANCH * D_MODEL

kernel_name = 'hybrid_mlstm_s5_fox_hmoe_encoder'


def _in_starts():
    return [int(s) for s in np.cumsum((0,) + IN_SIZES)]


def layer_norm(x, g, b):
    xf = x.astype(jnp.float32)
    mu = jnp.mean(xf, axis=-1, keepdims=True)
    var = jnp.mean(jnp.square(xf - mu), axis=-1, keepdims=True)
    return ((xf - mu) * lax.rsqrt(var + LN_EPS)).astype(x.dtype) * g + b


def head_norm(h, w, n_heads):
    bsz, seq, width = h.shape
    hf = h.reshape(bsz, seq, n_heads, width // n_heads).astype(jnp.float32)
    mu = jnp.mean(hf, axis=-1, keepdims=True)
    var = jnp.mean(jnp.square(hf - mu), axis=-1, keepdims=True)
    out = ((hf - mu) * lax.rsqrt(var + LN_EPS)).reshape(bsz, seq, width)
    return out.astype(h.dtype) * w


def causal_depthwise_conv(u, w, b):
    k = w.shape[0]
    out = lax.conv_general_dilated(
        u, w[:, None, :].astype(u.dtype), window_strides=(1,), padding=[(k - 1, 0)],
        dimension_numbers=('NWC', 'WIO', 'NWC'), feature_group_count=u.shape[-1])
    return out + b


def _to_chunks(t):
    bsz, seq, heads = t.shape[:3]
    t = t.reshape((bsz, seq // CHUNK, CHUNK, heads) + t.shape[3:])
    return jnp.moveaxis(t, (1, 3), (0, 2))


def mlstm_chunkwise(q, k, v, i_pre, f_pre):
    bsz, seq, heads, dh = q.shape
    f32 = jnp.float32
    qc = _to_chunks(q.astype(f32))
    kc = _to_chunks(k.astype(f32) * dh ** -0.5)
    vc = _to_chunks(v.astype(f32))
    ic = _to_chunks(i_pre.astype(f32))
    lfc = _to_chunks(jax.nn.log_sigmoid(f_pre.astype(f32)))
    causal = jnp.tril(jnp.ones((CHUNK, CHUNK), dtype=bool))

    def step(carry, inp):
        c_st, n_st, m_st = carry
        q_, k_, v_, i_, lf = inp
        b = jnp.cumsum(lf, axis=-1)
        d_log = jnp.where(causal, b[..., :, None] - b[..., None, :] + i_[..., None, :], -jnp.inf)
        g_log = b + m_st[..., None]
        m_t = jnp.maximum(jnp.max(d_log, axis=-1), g_log)
        w_intra = jnp.exp(d_log - m_t[..., None])
        w_state = jnp.exp(g_log - m_t)
        s = jnp.einsum('bhtd,bhsd->bhts', q_, k_) * w_intra
        num = (jnp.einsum('bhts,bhsd->bhtd', s, v_)
               + w_state[..., None] * jnp.einsum('bhtk,bhkv->bhtv', q_, c_st))
        den = jnp.sum(s, axis=-1) + w_state * jnp.einsum('bhtk,bhk->bht', q_, n_st)
        h = num / jnp.maximum(jnp.abs(den), jnp.exp(-m_t))[..., None]
        b_last = b[..., -1]
        a_log = b_last[..., None] - b + i_
        m_new = jnp.maximum(b_last + m_st, jnp.max(a_log, axis=-1))
        kw = k_ * jnp.exp(a_log - m_new[..., None])[..., None]
        decay = jnp.exp(b_last + m_st - m_new)
        c_new = decay[..., None, None] * c_st + jnp.einsum('bhsk,bhsv->bhkv', kw, v_)
        n_new = decay[..., None] * n_st + jnp.sum(kw, axis=2)
        return (c_new, n_new, m_new), h

    init = (jnp.zeros((bsz, heads, dh, dh), f32), jnp.zeros((bsz, heads, dh), f32),
            jnp.zeros((bsz, heads), f32))
    _, h = lax.scan(step, init, (qc, kc, vc, ic, lfc))
    h = jnp.moveaxis(h, (0, 2), (1, 3)).reshape(bsz, seq, heads * dh)
    return h.astype(q.dtype)


def s5_grouped(u, lam_re, lam_im, log_dt, b_re, b_im, c_re, c_im, d):
    f32 = jnp.float32
    bsz, seq, _ = u.shape
    ug = u.astype(f32).reshape(bsz, seq, S5_GROUPS, S5_GROUP)
    lr, li = lam_re.astype(f32), lam_im.astype(f32)
    dt = jnp.exp(log_dt.astype(f32))[:, None]
    mag = jnp.exp(lr * dt)
    abar_re, abar_im = mag * jnp.cos(li * dt), mag * jnp.sin(li * dt)
    den = lr * lr + li * li
    nr, ni = abar_re - 1.0, abar_im
    zr = (nr * lr + ni * li) / den
    zi = (ni * lr - nr * li) / den
    br, bi = b_re.astype(f32), b_im.astype(f32)
    bbar_re = zr[..., None] * br - zi[..., None] * bi
    bbar_im = zr[..., None] * bi + zi[..., None] * br
    xr = jnp.einsum('gpc,bsgc->bsgp', bbar_re, ug)
    xi = jnp.einsum('gpc,bsgc->bsgp', bbar_im, ug)
    a_re = jnp.broadcast_to(abar_re, (1, seq) + abar_re.shape)
    a_im = jnp.broadcast_to(abar_im, (1, seq) + abar_im.shape)

    def combine(e1, e2):
        a1r, a1i, b1r, b1i = e1
        a2r, a2i, b2r, b2i = e2
        return (a1r * a2r - a1i * a2i, a1r * a2i + a1i * a2r,
                a2r * b1r - a2i * b1i + b2r, a2r * b1i + a2i * b1r + b2i)

    _, _, hr, hi = lax.associative_scan(combine, (a_re, a_im, xr, xi), axis=1)
    y = (jnp.einsum('gcp,bsgp->bsgc', c_re.astype(f32), hr)
         - jnp.einsum('gcp,bsgp->bsgc', c_im.astype(f32), hi)
         + d.astype(f32) * ug)
    return y.reshape(bsz, seq, S5_GROUPS * S5_GROUP).astype(u.dtype)


def forgetting_attention(q, k, v, f_pre):
    bsz, seq, heads, dh = q.shape
    n_blk = seq // Q_BLOCK
    f_cum = jnp.cumsum(jax.nn.log_sigmoid(f_pre.astype(jnp.float32)), axis=1)
    f_cum = jnp.transpose(f_cum, (0, 2, 1))
    q_blk = jnp.moveaxis(q.reshape(bsz, n_blk, Q_BLOCK, heads, dh), 1, 0)
    f_blk = jnp.moveaxis(f_cum.reshape(bsz, heads, n_blk, Q_BLOCK), 2, 0)
    k_pos = jnp.arange(seq)
    scale = dh ** -0.5

    def one_block(args):
        qb, fb, bi = args
        q_pos = bi * Q_BLOCK + jnp.arange(Q_BLOCK)
        s = jnp.einsum('bqhd,bkhd->bhqk', qb, k).astype(jnp.float32) * scale
        s = s + fb[..., :, None] - f_cum[..., None, :]
        s = jnp.where(k_pos[None, :] <= q_pos[:, None], s, -jnp.inf)
        p = jax.nn.softmax(s, axis=-1).astype(v.dtype)
        return jnp.einsum('bhqk,bkhd->bqhd', p, v)

    out = lax.map(one_block, (q_blk, f_blk, jnp.arange(n_blk)))
    return jnp.moveaxis(out, 0, 1).reshape(bsz, seq, heads * dh)


def hier_moe(x, w_rg, b_rg, w_re, b_re, w_gate, w_up, w_down):
    bsz, seq, d = x.shape
    n_tok = bsz * seq
    xf = x.reshape(n_tok, d)
    lg = (xf @ w_rg).astype(jnp.float32) + b_rg.astype(jnp.float32)
    grp = jnp.argmax(lg, axis=-1)
    gate_g = jnp.take_along_axis(jax.nn.softmax(lg, axis=-1), grp[:, None], axis=-1)[:, 0]
    le = ((xf @ w_re).astype(jnp.float32) + b_re.astype(jnp.float32)).reshape(
        n_tok, N_GROUPS, EXPERTS_PER_GROUP)
    le = jnp.take_along_axis(le, grp[:, None, None], axis=1)[:, 0]
    top_l, top_i = lax.top_k(le, TOP_K)
    weights = gate_g[:, None] * jax.nn.softmax(top_l, axis=-1)
    expert = grp[:, None] * EXPERTS_PER_GROUP + top_i

    n_asg = n_tok * TOP_K
    flat_e = expert.reshape(n_asg)
    flat_t = jnp.repeat(jnp.arange(n_tok, dtype=jnp.int32), TOP_K)
    flat_w = weights.reshape(n_asg)
    order = jnp.argsort(flat_e)
    se = flat_e[order]
    counts = jnp.zeros((N_EXPERTS,), jnp.int32).at[flat_e].add(1)
    starts = jnp.cumsum(counts) - counts
    pcounts = (counts + MOE_BLOCK - 1) // MOE_BLOCK * MOE_BLOCK
    pends = jnp.cumsum(pcounts)
    pstarts = pends - pcounts
    dest = pstarts[se] + jnp.arange(n_asg, dtype=jnp.int32) - starts[se]
    n_blk = -(-n_asg // MOE_BLOCK) + N_EXPERTS
    buf_tok = jnp.full((n_blk * MOE_BLOCK,), n_tok, jnp.int32).at[dest].set(flat_t[order])
    buf_w = jnp.zeros((n_blk * MOE_BLOCK,), jnp.float32).at[dest].set(flat_w[order])
    blk_e = jnp.minimum(jnp.searchsorted(pends, jnp.arange(n_blk) * MOE_BLOCK, side='right'),
                        N_EXPERTS - 1)
    x_pad = jnp.concatenate([xf, jnp.zeros((1, d), xf.dtype)], axis=0)
    xb = x_pad[buf_tok].reshape(n_blk, MOE_BLOCK, d)

    def expert_block(args):
        xe, e = args
        h = jax.nn.silu(xe @ w_gate[e]) * (xe @ w_up[e])
        return h @ w_down[e]

    yb = lax.map(expert_block, (xb, blk_e)).reshape(n_blk * MOE_BLOCK, d)
    y = jax.ops.segment_sum(yb * buf_w[:, None].astype(yb.dtype), buf_tok,
                            num_segments=n_tok + 1)[:n_tok]
    return y.reshape(bsz, seq, d)


def setup_inputs(seed: int = 0) -> dict:
    key = jax.random.key(seed)
    ks = list(jax.random.split(key, 32))
    f32 = jnp.float32
    L, D, W = DEPTH, D_MODEL, BRANCH_WIDTH
    G, P, C = S5_GROUPS, S5_STATE, S5_GROUP
    E, F = N_EXPERTS, EXPERT_HIDDEN

    def nrm(i, shape, std):
        return jax.random.normal(ks[i], shape, f32) * std

    st = _in_starts()
    b_in = nrm(2, (L, IN_TOTAL), 0.02)
    b_in = b_in.at[:, st[4]:st[4] + M_HEADS].add(jnp.linspace(3.0, 6.0, M_HEADS, dtype=f32))
    b_in = b_in.at[:, st[7]:st[7] + A_HEADS].add(jnp.linspace(1.0, 5.0, A_HEADS, dtype=f32))
    return {
        'x': nrm(0, (BATCH, SEQ, D), 1.0),
        'w_in': nrm(1, (L, D, IN_TOTAL), D ** -0.5),
        'b_in': b_in,
        'm_conv_w': nrm(3, (L, M_CONV, 2 * W), M_CONV ** -0.5),
        'm_conv_b': nrm(4, (L, 2 * W), 0.02),
        'm_norm_w': 1.0 + nrm(5, (L, W), 0.02),
        's5_lam_re': -0.5 + nrm(6, (L, G, P), 0.01),
        's5_lam_im': math.pi * jnp.arange(P, dtype=f32) + nrm(7, (L, G, P), 0.01),
        's5_log_dt': jax.random.uniform(ks[8], (L, G), f32, math.log(1e-3), math.log(1e-1)),
        's5_b_re': nrm(9, (L, G, P, C), (2 * C) ** -0.5),
        's5_b_im': nrm(10, (L, G, P, C), (2 * C) ** -0.5),
        's5_c_re': nrm(11, (L, G, C, P), (2 * P) ** -0.5),
        's5_c_im': nrm(12, (L, G, C, P), (2 * P) ** -0.5),
        's5_d': nrm(13, (L, G, C), 1.0),
        's5_w_glu': nrm(14, (L, W, W), W ** -0.5),
        's5_b_glu': nrm(15, (L, W), 0.02),
        'w_branch': nrm(16, (L, N_BRANCH, W, D), W ** -0.5),
        'w_out': nrm(17, (L, D, D), BETA * D ** -0.5),
        'ln1_g': 1.0 + nrm(18, (L, D), 0.02),
        'ln1_b': nrm(19, (L, D), 0.02),
        'w_route_group': nrm(20, (L, D, N_GROUPS), D ** -0.5),
        'b_route_group': nrm(21, (L, N_GROUPS), 0.01),
        'w_route_expert': nrm(22, (L, D, N_EXPERTS), D ** -0.5),
        'b_route_expert': nrm(23, (L, N_EXPERTS), 0.01),
        'moe_w_gate': nrm(24, (L, E, D, F), D ** -0.5),
        'moe_w_up': nrm(25, (L, E, D, F), D ** -0.5),
        'moe_w_down': nrm(26, (L, E, F, D), BETA * F ** -0.5),
        'ln2_g': 1.0 + nrm(27, (L, D), 0.02),
        'ln2_b': nrm(28, (L, D), 0.02),
    }


def reference(x, w_in, b_in, m_conv_w, m_conv_b, m_norm_w, s5_lam_re, s5_lam_im, s5_log_dt,
              s5_b_re, s5_b_im, s5_c_re, s5_c_im, s5_d, s5_w_glu, s5_b_glu, w_branch, w_out,
              ln1_g, ln1_b, w_route_group, b_route_group, w_route_expert, b_route_expert,
              moe_w_gate, moe_w_up, moe_w_down, ln2_g, ln2_b):
    bsz, seq, d = x.shape
    cuts = _in_starts()[1:-1]
    m_shape = (bsz, seq, M_HEADS, M_HEAD_DIM)
    a_shape = (bsz, seq, A_HEADS, A_HEAD_DIM)
    for l in range(DEPTH):
        proj = x @ w_in[l] + b_in[l]
        m_qk, m_v, m_o, m_i, m_f, s_u, a_qkv, a_f, gate_pre = jnp.split(proj, cuts, axis=-1)

        m_qk = jax.nn.silu(causal_depthwise_conv(m_qk, m_conv_w[l], m_conv_b[l]))
        m_q, m_k = jnp.split(m_qk, 2, axis=-1)
        h_m = mlstm_chunkwise(m_q.reshape(m_shape), m_k.reshape(m_shape), m_v.reshape(m_shape),
                              m_i, m_f)
        y_m = jax.nn.sigmoid(m_o) * head_norm(h_m, m_norm_w[l], M_HEADS)

        y_s = jax.nn.gelu(s5_grouped(s_u, s5_lam_re[l], s5_lam_im[l], s5_log_dt[l], s5_b_re[l],
                                     s5_b_im[l], s5_c_re[l], s5_c_im[l], s5_d[l]))
        y_s = y_s * jax.nn.sigmoid(y_s @ s5_w_glu[l] + s5_b_glu[l])

        a_q, a_k, a_v = jnp.split(a_qkv, 3, axis=-1)
        y_a = forgetting_attention(a_q.reshape(a_shape), a_k.reshape(a_shape),
                                   a_v.reshape(a_shape), a_f)

        gates = jax.nn.sigmoid(gate_pre).reshape(bsz, seq, N_BRANCH, d)
        mixed = (gates[:, :, 0] * (y_m @ w_branch[l, 0])
                 + gates[:, :, 1] * (y_s @ w_branch[l, 1])
                 + gates[:, :, 2] * (y_a @ w_branch[l, 2]))
        x = layer_norm(ALPHA * x + mixed @ w_out[l], ln1_g[l], ln1_b[l])

        moe_out = hier_moe(x, w_route_group[l], b_route_group[l], w_route_expert[l],
                           b_route_expert[l], moe_w_gate[l], moe_w_up[l], moe_w_down[l])
        x = layer_norm(ALPHA * x + moe_out, ln2_g[l], ln2_b[l])
    return x
```

```python
import numpy as np
import concourse.bass as bass
import concourse.mybir as mybir
from concourse.bass_utils import run_bass_kernel_spmd

F32 = mybir.dt.float32
BF16 = mybir.dt.bfloat16
I32 = mybir.dt.int32
U32 = mybir.dt.uint32
AF = mybir.ActivationFunctionType
ALU = mybir.AluOpType
AX = mybir.AxisListType


class Res:
    __slots__ = ("name", "last_w", "readers", "ap")

    def __init__(self, name, ap=None):
        self.name = name
        self.last_w = None
        self.readers = {}
        self.ap = ap


class Eng:
    def __init__(self, name, handle, sem, semkey):
        self.name = name
        self.h = handle
        self.sem = sem
        self.semkey = semkey
        self.count = 0
        self.seen = {}
        self.ring = []
        self.dma_count = 0


class FW:
    RING = 8

    def __init__(self, nc):
        self.nc = nc
        self.sems = {}
        self._ctx = []
        self.n_instr = 0

        def mk(name):
            cm = nc.semaphore(name)
            s = cm.__enter__()
            self._ctx.append(cm)
            self.sems[name] = s
            return s

        self.PE = Eng("pe", nc.tensor, mk("s_pe"), "s_pe")
        self.DVE = Eng("dve", nc.vector, mk("s_dve"), "s_dve")
        self.ACT = Eng("act", nc.scalar, mk("s_act"), "s_act")
        self.POOL = Eng("pool", nc.gpsimd, mk("s_pool"), "s_pool")
        self.SP = Eng("sp", nc.sync, mk("s_sp"), "s_sp")
        for e in (self.SP, self.ACT, self.POOL):
            for i in range(4 if e is self.POOL else self.RING):
                k = "r_%s_%d" % (e.name, i)
                mk(k)
                e.ring.append(k)
        self.engines = [self.PE, self.DVE, self.ACT, self.POOL, self.SP]

    def res(self, name, ap=None):
        return Res(name, ap)

    def sb(self, name, shape, dtype):
        self._uid = getattr(self, '_uid', 0) + 1
        name = "%s_u%d" % (name, self._uid)
        cm = self.nc.sbuf_tensor(name, shape, dtype)
        t = cm.__enter__()
        self._ctx.append(cm)
        return Res(name, t)

    def ps(self, name, shape, dtype):
        cm = self.nc.psum_tensor(name, shape, dtype)
        t = cm.__enter__()
        self._ctx.append(cm)
        return Res(name, t)

    def _wait(self, eng, deps):
        best = {}
        for d in deps:
            if d is None:
                continue
            k, v = d
            if best.get(k, 0) < v:
                best[k] = v
        for k, v in best.items():
            if eng is self.PE and k == "s_pe":
                continue
            if eng.seen.get(k, 0) < v:
                eng.h.wait_ge(self.sems[k], v)
                eng.seen[k] = v

    def _deps(self, reads, writes):
        deps = []
        for r in reads:
            deps.append(r.last_w)
        for w in writes:
            deps.append(w.last_w)
            deps.extend(w.readers.items())
        return deps

    def _commit(self, ev, reads, writes):
        k, v = ev
        for r in reads:
            if r.readers.get(k, 0) < v:
                r.readers[k] = v
        for w in writes:
            w.last_w = ev
            w.readers = {}

    def op(self, eng, fn, reads=(), writes=()):
        if getattr(self, '_defer', None) is not None:
            self._defer.append((self._op_now, (eng, fn, tuple(reads), tuple(writes)), {}))
            return None
        return self._op_now(eng, fn, reads, writes)

    def _op_now(self, eng, fn, reads=(), writes=()):
        self._wait(eng, self._deps(reads, writes))
        ins = fn()
        eng.count += 1
        ins.then_inc(eng.sem, 1)
        self.n_instr += 1
        self._commit((eng.semkey, eng.count), reads, writes)
        return ins

    def dma(self, q, out, in_, reads=(), writes=(), fn=None, **kw):
        if getattr(self, '_defer', None) is not None:
            kw2 = dict(kw)
            kw2.update(reads=tuple(reads), writes=tuple(writes), fn=fn)
            self._defer.append((self._dma_now, (q, out, in_), kw2))
            return None
        return self._dma_now(q, out, in_, reads=reads, writes=writes, fn=fn, **kw)

    def _dma_now(self, q, out, in_, reads=(), writes=(), fn=None, **kw):
        deps = self._deps(reads, writes)
        k = q.dma_count
        nr = len(q.ring)
        slot = q.ring[k % nr]
        rnd = k // nr
        if rnd > 0:
            deps.append((slot, 16 * rnd))
        self._wait(q, deps)
        if fn is None:
            ins = q.h.dma_start(out=out, in_=in_, **kw)
        else:
            ins = fn()
        ins.then_inc(self.sems[slot], 16)
        q.dma_count += 1
        self.n_instr += 1
        self._commit((slot, 16 * (rnd + 1)), reads, writes)
        return ins

    def finish(self, outs):
        deps = []
        for o in outs:
            deps.append(o.last_w)
        for e in (self.SP, self.ACT, self.POOL):
            k = e.dma_count
            nr = len(e.ring)
            for i in range(min(k, nr)):
                j = k - 1 - i
                deps.append((e.ring[j % nr], 16 * (j // nr + 1)))
        for e in (self.PE, self.DVE, self.ACT, self.POOL):
            if e.count:
                deps.append((e.semkey, e.count))
        self._wait(self.SP, deps)


S = 4096
D = 1024
W = 512
NT = 32
NCH = 8
IN_TOTAL = 7184
ST = [0, 1024, 1536, 2048, 2052, 2056, 2568, 4104, 4112, 7184]
DEPTH = 4
ALPHA = (2 * DEPTH) ** 0.25
LN_EPS = 1e-5
NE = 32
CAP = 384
NTB = CAP // 128
CS = CAP + 128
TWO_PI = 6.283185307179586

INPUT_NAMES = ['w_in', 'b_in', 'm_conv_w', 'm_conv_b', 'm_norm_w', 's5_lam_re', 's5_lam_im',
               's5_log_dt', 's5_b_re', 's5_b_im', 's5_c_re', 's5_c_im', 's5_d', 's5_w_glu',
               's5_b_glu', 'w_branch', 'w_out', 'ln1_g', 'ln1_b', 'w_route_group',
               'b_route_group', 'w_route_expert', 'b_route_expert', 'moe_w_gate', 'moe_w_up',
               'moe_w_down', 'ln2_g', 'ln2_b']
INPUT_SHAPES = {
    'w_in': (1024, 7184), 'b_in': (7184,), 'm_conv_w': (4, 1024), 'm_conv_b': (1024,),
    'm_norm_w': (512,), 's5_lam_re': (32, 64), 's5_lam_im': (32, 64), 's5_log_dt': (32,),
    's5_b_re': (32, 64, 16), 's5_b_im': (32, 64, 16), 's5_c_re': (32, 16, 64),
    's5_c_im': (32, 16, 64), 's5_d': (32, 16), 's5_w_glu': (512, 512), 's5_b_glu': (512,),
    'w_branch': (3, 512, 1024), 'w_out': (1024, 1024), 'ln1_g': (1024,), 'ln1_b': (1024,),
    'w_route_group': (1024, 4), 'b_route_group': (4,), 'w_route_expert': (1024, 32),
    'b_route_expert': (32,), 'moe_w_gate': (32, 1024, 512), 'moe_w_up': (32, 1024, 512),
    'moe_w_down': (32, 512, 1024), 'ln2_g': (1024,), 'ln2_b': (1024,),
}


def host_consts():
    import ml_dtypes
    c = {}
    c['c_ident_bf'] = np.eye(128, dtype=np.float32).astype(ml_dtypes.bfloat16)
    c['c_ident_f'] = np.eye(128, dtype=np.float32)
    s_idx = np.arange(128)[:, None]
    t_idx = np.arange(128)[None, :]
    m01 = (s_idx <= t_idx).astype(np.float32)
    c['c_mask01x4'] = np.tile(m01, (1, 4))
    c['c_maskneg'] = np.where(t_idx <= s_idx, 0.0, -30000.0).astype(np.float32)
    c['c_ustrict'] = (s_idx < t_idx).astype(np.float32).astype(ml_dtypes.bfloat16)
    c['c_ones_bf'] = np.ones((128, 128), np.float32).astype(ml_dtypes.bfloat16)
    sel = np.zeros((8, 8 * 128), np.float32)
    for h in range(8):
        sel[h, h * 128:(h + 1) * 128] = 1.0
    c['c_sel8'] = sel
    c['c_iota'] = np.tile(np.arange(S, dtype=np.float32)[None, :], (128, 1))
    esel = np.zeros((128, 8, 16), np.float32)
    for k in range(128):
        esel[k, k // 16, k % 16] = 1.0
    c['c_esel'] = esel
    c['c_ecap'] = np.tile((np.arange(NE, dtype=np.float32) * CS)[None, :], (128, 1))
    return c


CONST_SPECS = {
    'c_ident_bf': ([128, 128], BF16), 'c_ident_f': ([128, 128], F32),
    'c_mask01x4': ([128, 512], F32), 'c_maskneg': ([128, 128], F32),
    'c_ustrict': ([128, 128], BF16), 'c_ones_bf': ([128, 128], BF16),
    'c_sel8': ([8, 1024], F32), 'c_iota': ([128, S], F32), 'c_esel': ([128, 8, 16], F32),
    'c_ecap': ([128, NE], F32),
}


class Prog:
    def __init__(self, L, debug=(), stop_after=None):
        self.L = L
        self.debug = set(debug)
        self.stop_after = stop_after
        nc = self.nc = bass.Bass("TRN2", target_bir_lowering=False)
        fw = self.fw = FW(nc)
        self.PE, self.DVE, self.ACT, self.POOL, self.SP = fw.PE, fw.DVE, fw.ACT, fw.POOL, fw.SP
        self.inp = {}
        self.x_in = nc.dram_tensor("x", [S, D], F32, kind="ExternalInput").ap()
        self.R_xin = fw.res("x")
        for n in INPUT_NAMES:
            self.inp[n] = nc.dram_tensor(n, [L] + list(INPUT_SHAPES[n]), F32, kind="ExternalInput").ap()
        self.R_w = fw.res("weights")
        self.cst = {}
        for n, (shp, dt) in CONST_SPECS.items():
            self.cst[n] = nc.dram_tensor(n, shp, dt, kind="ExternalInput").ap()
        self.out = nc.dram_tensor("out", [S, D], F32, kind="ExternalOutput").ap()
        self.R_out = fw.res("out")
        self.scr = {}
        self.R = {}
        for n, shp, dt in [
            ("xT", [D, S], BF16), ("xres1", [S, D], F32), ("xres0", [S, D], F32),
            ("x1T", [D, S], BF16), ("x1b", [S, D], BF16),
            ("qkT", [1024, S], BF16), ("moT", [512, S], F32), ("suT", [512, S], BF16),
            ("aqT", [512, S], BF16), ("akT", [512, S], BF16), ("gT", [3072, S], F32),
            ("gsm", [16, S], F32), ("vm", [S, 512], BF16), ("va", [S, 512], BF16),
            ("ymT", [512, S], BF16), ("ysT", [512, S], BF16), ("yaT", [512, S], BF16),
            ("s5y", [512, S], F32), ("fx", [4, 8, S], BF16),
            ("xdisp", [NE * CS, D], BF16), ("ydisp", [NE * CS, D], F32), ("rinfo", [128, NT * 4], F32),
        ]:
            kind = "ExternalOutput" if n in self.debug else "Internal"
            self.scr[n] = nc.dram_tensor(n, shp, dt, kind=kind).ap()
            self.R[n] = fw.res(n)
        self.ident_bf = fw.sb("ident_bf", [128, 128], BF16)
        self.ident_f = fw.sb("ident_f", [128, 128], F32)
        self.ones_bf = fw.sb("ones_bf", [128, 128], BF16)
        self.R_c = fw.res("consts")
        self.eps_col = fw.sb("eps_col", [128, 1], F32)
        self.memset(self.DVE, self.eps_col.ap[:], LN_EPS, [self.eps_col])
        self.ld(self.ident_bf.ap[:], self.cst['c_ident_bf'], [self.R_c], [self.ident_bf])
        self.ld(self.ident_f.ap[:], self.cst['c_ident_f'], [self.R_c], [self.ident_f])
        self.ld(self.ones_bf.ap[:], self.cst['c_ones_bf'], [self.R_c], [self.ones_bf])
        self.banks = [fw.ps("bank%d" % i, [128, 512], F32) for i in range(8)]
        self._bank_i = 0
        self._marks = []

    def bank(self):
        b = self.banks[self._bank_i % 8]
        self._bank_i += 1
        return b

    def push(self):
        self._marks.append(len(self.fw._ctx))

    def pop(self):
        self.barrier()
        m = self._marks.pop()
        while len(self.fw._ctx) > m:
            cm = self.fw._ctx.pop()
            cm.__exit__(None, None, None)

    def barrier(self):
        fw = self.fw
        deps = []
        for e in (fw.SP, fw.ACT, fw.POOL):
            k = e.dma_count
            nr = len(e.ring)
            for i in range(min(k, nr)):
                j = k - 1 - i
                deps.append((e.ring[j % nr], 16 * (j // nr + 1)))
        for e in (fw.PE, fw.DVE, fw.ACT, fw.POOL):
            if e.count:
                deps.append((e.semkey, e.count))
        for e in fw.engines:
            fw._wait(e, deps)

    def ld(self, out, in_, reads, writes, q=None, **kw):
        return self.fw.dma(q or self.SP, out, in_, reads=reads, writes=writes, **kw)

    def mm(self, out, lhsT, rhs, start, stop, reads, writes):
        nc = self.nc
        return self.fw.op(self.PE, lambda: nc.tensor.matmul(out, lhsT, rhs, start=start, stop=stop),
                          reads, writes)

    def tr(self, out, in_, ident, reads, writes):
        nc = self.nc
        return self.fw.op(self.PE, lambda: nc.tensor.transpose(out, in_, ident), reads, writes)

    def act(self, out, in_, func, reads, writes, bias=None, scale=1.0, accum=None):
        nc = self.nc
        kw = {}
        if bias is not None:
            kw['bias'] = bias
        if accum is not None:
            kw['accum_out'] = accum
        return self.fw.op(self.ACT, lambda: nc.scalar.activation(out=out, in_=in_, func=func, scale=scale, **kw),
                          reads, writes)

    def tt(self, eng, out, in0, in1, op, reads, writes):
        return self.fw.op(eng, lambda: eng.h.tensor_tensor(out=out, in0=in0, in1=in1, op=op), reads, writes)

    def ts(self, eng, out, in0, s1, s2, op0, op1, reads, writes, accum=None):
        kw = {}
        if accum is not None:
            kw['accum_out'] = accum
        if s2 is None:
            return self.fw.op(eng, lambda: eng.h.tensor_scalar(out=out, in0=in0, scalar1=s1, scalar2=None, op0=op0, **kw),
                              reads, writes)
        return self.fw.op(eng, lambda: eng.h.tensor_scalar(out=out, in0=in0, scalar1=s1, scalar2=s2, op0=op0, op1=op1, **kw),
                          reads, writes)

    def stt(self, eng, out, in0, scalar, in1, op0, op1, reads, writes):
        return self.fw.op(eng, lambda: eng.h.scalar_tensor_tensor(out=out, in0=in0, scalar=scalar, in1=in1, op0=op0, op1=op1),
                          reads, writes)

    def cp(self, eng, out, in_, reads, writes):
        if eng is self.ACT:
            return self.act(out, in_, AF.Copy, reads, writes)
        return self.fw.op(eng, lambda: eng.h.tensor_copy(out=out, in_=in_), reads, writes)

    def memset(self, eng, ap, val, writes):
        return self.fw.op(eng, lambda: eng.h.memset(ap, val), (), writes)

    def red(self, eng, out, in_, op, reads, writes, axis=None):
        axis = axis or AX.X
        return self.fw.op(eng, lambda: eng.h.tensor_reduce(out=out, in_=in_, axis=axis, op=op), reads, writes)

    def col_load(self, tile_res, col, vec_ap, n):
        self.ld(tile_res.ap[0:n, col:col + 1], vec_ap.rearrange("(p o) -> p o", o=1), [self.R_w], [tile_res])

    def transpose_store(self, src_bf, src_res, dstT, dstT_res, i, stage, stage_i, pool=None):
        b = self.bank_from(*pool) if pool else self.bank()
        pv = b.ap[:].bitcast(BF16)
        for kt in range(8):
            self.tr(pv[:, kt * 128:(kt + 1) * 128], src_bf[:, kt * 128:(kt + 1) * 128], self.ident_bf.ap[:],
                    [src_res, self.ident_bf], [b])
        st = stage[stage_i % len(stage)]
        self.cp(self.ACT, st.ap[:], pv, [b], [st])
        self.ld(dstT.rearrange("(kt p) t -> p kt t", p=128)[:, :, i * 128:(i + 1) * 128],
                st.ap[:].rearrange("p (kt t) -> p kt t", kt=8), [st], [dstT_res])

    def phase_t0(self):
        self.push()
        fw = self.fw
        xt = [fw.sb("t0x%d" % i, [128, D], F32) for i in range(3)]
        xb = [fw.sb("t0b%d" % i, [128, D], BF16) for i in range(2)]
        stg = [fw.sb("t0s%d" % i, [128, D], BF16) for i in range(2)]

        def A(i):
            t = xt[i % 3]
            self.ld(t.ap[:], self.x_in[i * 128:(i + 1) * 128, :], [self.R_xin], [t])

        def B(i):
            t = xt[i % 3]
            self.ld(self.scr['xres0'][i * 128:(i + 1) * 128, :], t.ap[:], [t], [self.R['xres0']])
            self.cp(self.DVE, xb[i % 2].ap[:], t.ap[:], [t], [xb[i % 2]])

        def C(i):
            self.transpose_store(xb[i % 2].ap, xb[i % 2], self.scr['xT'], self.R['xT'], i, stg, i)

        self.pipeline(NT, [A, B, C])
        self.pop()

    def phase_inproj(self, l):
        self.push()
        fw, nc = self.fw, self.nc
        w_in = self.inp['w_in'][l]
        b_in = self.inp['b_in'][l]
        xT = fw.sb("p1_xT", [128, 8, S], BF16)
        self.ld(xT.ap[:], self.scr['xT'].rearrange("(kt p) t -> p kt t", p=128), [self.R['xT']], [xT])
        wbf = [fw.sb("p1_wbf%d" % i, [128, 8, 128], BF16) for i in range(3)]
        rowf = fw.sb("p1_rowf", [128, S + 4], F32)
        acc = fw.sb("p1_acc", [128, S], F32)
        rowo = [fw.sb("p1_rowo%d" % i, [128, S], F32) for i in range(2)]
        bcol = fw.sb("p1_bcol", [128, 64], F32)
        cw = fw.sb("p1_cw", [128, 8, 4], F32)
        cb = fw.sb("p1_cb", [128, 8], F32)
        self.memset(self.DVE, rowf.ap[:, 0:4], 0.0, [rowf])
        for j in range(4):
            self.ld(cw.ap[:, :, j], self.inp['m_conv_w'][l][j].rearrange("(t p) -> p t", p=128), [self.R_w], [cw],
                    allow_slow_non_contiguous=True)
        self.ld(cb.ap[:], self.inp['m_conv_b'][l].rearrange("(t p) -> p t", p=128), [self.R_w], [cb],
                allow_slow_non_contiguous=True)
        tiles = []
        for t in range(8):
            tiles.append((ST[0] + t * 128, 128, "conv", "qkT", t * 128, t))
        for t in range(4):
            tiles.append((ST[2] + t * 128, 128, "sig32", "moT", t * 128, 0))
        for t in range(4):
            tiles.append((ST[5] + t * 128, 128, "bf", "suT", t * 128, 0))
        for t in range(4):
            tiles.append((ST[6] + t * 128, 128, "bfq", "aqT", t * 128, 0))
        for t in range(4):
            tiles.append((ST[6] + 512 + t * 128, 128, "bf", "akT", t * 128, 0))
        for t in range(24):
            tiles.append((ST[8] + t * 128, 128, "sig32", "gT", t * 128, 0))
        tiles.append((None, 16, "small", "gsm", 0, 0))
        for ti, (c0, n, kind, dn, r0, aux) in enumerate(tiles):
            if kind == "small":
                self.col_load(bcol, ti, b_in[ST[3]:ST[3] + 8], 8)
                self.ld(bcol.ap[8:16, ti:ti + 1], b_in[ST[7]:ST[7] + 8].rearrange("(p o) -> p o", o=1),
                        [self.R_w], [bcol])
            else:
                self.col_load(bcol, ti, b_in[c0:c0 + n], n)
        bq = fw.sb("p1_bq", [128, 4], F32)

        def load_w(ti):
            c0, n, kind, dn, r0, aux = tiles[ti]
            ws = wbf[ti % 3]
            if kind == "small":
                self.ld(ws.ap[:, :, 0:8], w_in[:, ST[3]:ST[3] + 8].rearrange("(kt p) c -> p kt c", p=128),
                        [self.R_w], [ws], q=self.POOL)
                self.ld(ws.ap[:, :, 8:16], w_in[:, ST[7]:ST[7] + 8].rearrange("(kt p) c -> p kt c", p=128),
                        [self.R_w], [ws], q=self.POOL)
            else:
                self.ld(ws.ap[:, :, 0:n], w_in[:, c0:c0 + n].rearrange("(kt p) c -> p kt c", p=128),
                        [self.R_w], [ws], q=self.POOL)

        load_w(0)
        load_w(1)
        for ti, (c0, n, kind, dn, r0, aux) in enumerate(tiles):
            if ti + 2 < len(tiles):
                load_w(ti + 2)
            wb = wbf[ti % 3]
            ro = rowo[ti % 2]
            ro_bf = ro.ap[:].bitcast(BF16)
            bc = bcol.ap[0:n, ti:ti + 1]
            for tc in range(NCH):
                b = self.bank()
                for kt in range(8):
                    self.mm(b.ap[0:n, :], wb.ap[:, kt, 0:n], xT.ap[:, kt, tc * 512:(tc + 1) * 512],
                            kt == 0, kt == 7, [wb, xT], [b])
                sl = slice(tc * 512, (tc + 1) * 512)
                if kind == "conv":
                    self.act(rowf.ap[0:n, 3 + tc * 512: 3 + (tc + 1) * 512], b.ap[0:n, :], AF.Identity,
                             [b, bcol], [rowf], bias=bc)
                elif kind == "sig32":
                    self.act(ro.ap[0:n, sl], b.ap[0:n, :], AF.Sigmoid, [b, bcol], [ro], bias=bc)
                elif kind == "bf":
                    self.act(ro_bf[0:n, sl], b.ap[0:n, :], AF.Identity, [b, bcol], [ro], bias=bc)
                elif kind == "bfq":
                    if tc == 0:
                        self.ts(self.DVE, bq.ap[:, 0:1], bc, 0.125, None, ALU.mult, None, [bcol], [bq])
                    self.act(ro_bf[0:n, sl], b.ap[0:n, :], AF.Identity, [b, bq], [ro], bias=bq.ap[:, 0:1], scale=0.125)
                elif kind == "small":
                    self.act(ro.ap[0:n, sl], b.ap[0:n, :], AF.Identity, [b, bcol], [ro], bias=bc)
            dst = self.scr[dn]
            if kind == "conv":
                t = aux
                self.ts(self.DVE, acc.ap[:], rowf.ap[:, 0:S], cw.ap[:, t, 0:1], cb.ap[:, t:t + 1], ALU.mult, ALU.add,
                        [rowf, cw, cb], [acc])
                for j in range(1, 4):
                    self.stt(self.DVE, acc.ap[:], rowf.ap[:, j:j + S], cw.ap[:, t, j:j + 1], acc.ap[:], ALU.mult, ALU.add,
                             [rowf, cw, acc], [acc])
                self.act(ro_bf[:, 0:S], acc.ap[:], AF.Silu, [acc], [ro])
                self.ld(dst[r0:r0 + 128, :], ro_bf[:, 0:S], [ro], [self.R[dn]])
            elif kind in ("bf", "bfq"):
                self.ld(dst[r0:r0 + n, :], ro_bf[0:n, 0:S], [ro], [self.R[dn]])
            else:
                self.ld(dst[r0:r0 + n, :], ro.ap[0:n, :], [ro], [self.R[dn]])
        wvb = [fw.sb("p1_wvb%d" % i, [128, 8, 512], BF16) for i in range(2)]
        bvb = [fw.sb("p1_bvb%d" % i, [128, 512], F32) for i in range(2)]
        vout = [fw.sb("p1_vout%d" % i, [128, 512], BF16) for i in range(2)]
        for vi, (c0, dn) in enumerate([(ST[1], "vm"), (ST[6] + 1024, "va")]):
            self.ld(wvb[vi].ap[:], w_in[:, c0:c0 + 512].rearrange("(kt p) c -> p kt c", p=128), [self.R_w], [wvb[vi]],
                    q=self.POOL)
            self.ld(bvb[vi].ap[:], b_in[c0:c0 + 512].partition_broadcast(128), [self.R_w], [bvb[vi]])
            for i in range(NT):
                b = self.bank()
                for kt in range(8):
                    self.mm(b.ap[:], xT.ap[:, kt, i * 128:(i + 1) * 128], wvb[vi].ap[:, kt, :], kt == 0, kt == 7,
                            [xT, wvb[vi]], [b])
                vo = vout[i % 2]
                self.tt(self.DVE, vo.ap[:], b.ap[:], bvb[vi].ap[:], ALU.add, [b, bvb[vi]], [vo])
                self.ld(self.scr[dn][i * 128:(i + 1) * 128, :], vo.ap[:], [vo], [self.R[dn]])
        self.pop()

    def finish(self):
        self.fw.finish([self.R_out])
        return self.nc


def _phase_mlstm(self, l):
    self.push()
    fw, nc = self.fw, self.nc
    DVE, ACT, POOL, PE = self.DVE, self.ACT, self.POOL, self.PE
    sel = fw.sb("m_sel", [4, 512], F32)
    self.ld(sel.ap[:], self.cst['c_sel8'][0:4, 0:512], [self.R_c], [sel])
    mask4 = fw.sb("m_mask4", [128, 512], F32)
    self.ld(mask4.ap[:], self.cst['c_mask01x4'], [self.R_c], [mask4])
    normw = fw.sb("m_normw", [128, 4], F32)
    self.ld(normw.ap[:], self.inp['m_norm_w'][l].rearrange("(h p) -> p h", p=128), [self.R_w], [normw],
            allow_slow_non_contiguous=True)
    QT = fw.sb("m_QT", [128, 4, S], BF16)
    KT = fw.sb("m_KT", [128, 4, S], BF16)
    self.ld(QT.ap[:], self.scr['qkT'][0:512, :].rearrange("(h p) t -> p h t", p=128), [self.R['qkT']], [QT])
    self.ld(KT.ap[:], self.scr['qkT'][512:1024, :].rearrange("(h p) t -> p h t", p=128), [self.R['qkT']], [KT])
    eBb = fw.sb("m_eBb", [128, 4, NT], F32)
    self.push()
    Gi = fw.sb("m_Gi", [4, S], F32)
    Gf = fw.sb("m_Gf", [4, S], F32)
    self.ld(Gi.ap[:], self.scr['gsm'][0:4, :], [self.R['gsm']], [Gi])
    self.ld(Gf.ap[:], self.scr['gsm'][4:8, :], [self.R['gsm']], [Gf])
    E = Gf
    RM = fw.sb("m_RM", [4, S], F32)
    B = fw.sb("m_B", [4, S], F32)
    EQ = fw.sb("m_EQ", [4, S], F32)
    EK = Gi
    EB = fw.sb("m_EB", [4, NT], F32)
    self.act(E.ap[:], Gf.ap[:], AF.Exp, [Gf], [E], scale=-1.0)
    self.act(E.ap[:], E.ap[:], AF.Ln, [E], [E], bias=1.0)
    self.memset(DVE, RM.ap[:], 1.0, [RM])
    self.memset(DVE, RM.ap[:].rearrange("p (c s) -> p c s", s=128)[:, :, 0:1], 0.0, [RM])
    fw.op(DVE, lambda: nc.vector.tensor_tensor_scan(out=B.ap[:], data0=RM.ap[:], data1=E.ap[:], initial=0.0,
                                                    op0=ALU.mult, op1=ALU.subtract), [RM, E], [B])
    self.act(EQ.ap[:], B.ap[:], AF.Exp, [B], [EQ])
    self.tt(DVE, EK.ap[:], Gi.ap[:], B.ap[:], ALU.subtract, [Gi, B], [EK])
    self.act(EK.ap[:], EK.ap[:], AF.Exp, [EK], [EK], bias=float(np.log(128.0 ** -0.5)))
    self.cp(DVE, EB.ap[:], EQ.ap[:].rearrange("p (c s) -> p c s", s=128)[:, :, 127], [EQ], [EB])
    for h in range(4):
        b = self.bank()
        self.mm(b.ap[:, 0:NT], sel.ap[0:4, h * 128:(h + 1) * 128], EB.ap[0:4, :], True, True, [sel, EB], [b])
        self.cp(DVE, eBb.ap[:, h, :], b.ap[:, 0:NT], [b], [eBb])
        for (src, T) in ((EQ, QT), (EK, KT)):
            for tc in range(NCH):
                b = self.bank()
                sl = slice(tc * 512, (tc + 1) * 512)
                self.mm(b.ap[:], sel.ap[0:4, h * 128:(h + 1) * 128], src.ap[0:4, sl], True, True, [sel, src], [b])
                self.tt(DVE, T.ap[:, h, sl], T.ap[:, h, sl], b.ap[:], ALU.mult, [T, b], [T])
    self.pop()
    Vaug = fw.sb("m_V", [128, NT, 4, 129], BF16)
    for h in range(4):
        self.ld(Vaug.ap[:, :, h, 0:128], self.scr['vm'][:, h * 128:(h + 1) * 128].rearrange("(nt p) c -> p nt c", p=128),
                [self.R['vm']], [Vaug])
    self.memset(POOL, Vaug.ap[:, :, :, 128:129], 1.0, [Vaug])
    P32 = fw.sb("m_P32", [128, 4, 129], F32)
    Cbf = [fw.sb("m_Cbf%d" % i, [128, 4, 129], BF16) for i in range(3)]
    self.memset(DVE, P32.ap[:], 0.0, [P32])
    self.memset(DVE, Cbf[0].ap[:], 0.0, [Cbf[0]])
    Ktok = [fw.sb("m_Ktok%d" % i, [128, 512], BF16) for i in range(2)]
    STb = [fw.sb("m_STb%d" % i, [128, 512], BF16) for i in range(3)]
    h32 = [fw.sb("m_h32%d" % i, [128, 4, 128], F32) for i in range(6)]
    sq = [fw.sb("m_sq%d" % i, [128, 4, 128], F32) for i in range(2)]
    hnb = [fw.sb("m_hnb%d" % i, [128, 4, 128], BF16) for i in range(2)]
    st2 = [fw.sb("m_st2%d" % i, [128, 16], F32) for i in range(2)]
    st3 = [fw.sb("m_st3%d" % i, [128, 16], F32) for i in range(6)]
    mo = [fw.sb("m_mo%d" % i, [128, 4, 128], F32) for i in range(2)]
    ymo = [fw.sb("m_ymo%d" % i, [128, 4, 128], BF16) for i in range(2)]
    moT_v = self.scr['moT'].rearrange("(h p) t -> p h t", p=128)
    ymT_v = self.scr['ymT'].rearrange("(h p) t -> p h t", p=128)
    bDs = {}

    def M0(c):
        csl = slice(c * 128, (c + 1) * 128)
        bT = self.bank_from('mT0', [0])
        bTv = bT.ap[:].bitcast(BF16)
        for h in range(4):
            self.tr(bTv[:, h * 128:(h + 1) * 128], KT.ap[:, h, csl], self.ident_bf.ap[:], [KT, self.ident_bf], [bT])
        kt_ = Ktok[c % 2]
        self.cp(ACT, kt_.ap[:], bTv[:, 0:512], [bT], [kt_])
        bS = self.bank_from('mS', [1])
        for h in range(4):
            self.mm(bS.ap[:, h * 128:(h + 1) * 128], KT.ap[:, h, csl], QT.ap[:, h, csl], True, True, [KT, QT], [bS])
        stb = STb[c % 3]
        self.tt(DVE, stb.ap[:], bS.ap[:], mask4.ap[:], ALU.mult, [bS, mask4], [stb])
        bD = [self.bank_from('mD', [2, 3]), self.bank_from('mD', [2, 3])]
        bDs[c] = bD
        for h in range(4):
            o = bD[h // 2].ap[:, (h % 2) * 129:(h % 2) * 129 + 129]
            self.mm(o, kt_.ap[:, h * 128:(h + 1) * 128], Vaug.ap[:, c, h, :], True, True, [kt_, Vaug], [bD[h // 2]])

    def M1(c):
        bD = bDs.pop(c)
        cn = Cbf[(c + 1) % 3]
        for h in range(4):
            o = bD[h // 2].ap[:, (h % 2) * 129:(h % 2) * 129 + 129]
            cprev = max(c - 1, 0)
            self.stt(DVE, P32.ap[:, h, :], P32.ap[:, h, :], eBb.ap[:, h, cprev:cprev + 1], o, ALU.mult, ALU.add,
                     [P32, eBb, bD[h // 2]], [P32])
            self.act(cn.ap[:, h, :], P32.ap[:, h, :], AF.Identity, [P32, eBb], [cn], scale=eBb.ap[:, h, c:c + 1])

    def M2(c):
        csl = slice(c * 128, (c + 1) * 128)
        stb = STb[c % 3]
        cc = Cbf[c % 3]
        st = st2[c % 2]
        h_ = h32[c % 6]
        bN = [self.bank_from('mN', [4, 5]), self.bank_from('mN', [4, 5])]
        for h in range(4):
            o = bN[h // 2].ap[:, (h % 2) * 129:(h % 2) * 129 + 129]
            self.mm(o, stb.ap[:, h * 128:(h + 1) * 128], Vaug.ap[:, c, h, :], True, False, [stb, Vaug], [bN[h // 2]])
            self.mm(o, QT.ap[:, h, csl], cc.ap[:, h, :], False, True, [QT, cc], [bN[h // 2]])
        for j in range(2):
            den = bN[j].ap[:, 0:258].rearrange("p (a b) -> p a b", b=129)[:, :, 128]
            self.ts(DVE, st.ap[:, 8 + 2 * j:10 + 2 * j], den, -1.0, None, ALU.mult, None, [bN[j]], [st])
            self.tt(DVE, st.ap[:, 2 * j:2 * j + 2], den, st.ap[:, 8 + 2 * j:10 + 2 * j], ALU.max, [bN[j], st], [st])
        self.ts(DVE, st.ap[:, 0:4], st.ap[:, 0:4], 1.0, None, ALU.max, None, [st], [st])
        fw.op(DVE, lambda: nc.vector.reciprocal(out=st.ap[:, 4:8], in_=st.ap[:, 0:4]), [st], [st])
        for h in range(4):
            o = bN[h // 2].ap[:, (h % 2) * 129:(h % 2) * 129 + 128]
            self.act(h_.ap[:, h, :], o, AF.Identity, [bN[h // 2], st], [h_], scale=st.ap[:, 4 + h:5 + h])

    def M3a(c):
        st = st3[c % 6]
        h_ = h32[c % 6]
        self.red(DVE, st.ap[:, 0:4], h_.ap[:], ALU.add, [h_], [st])
        self.tt(POOL, sq[c % 2].ap[:], h_.ap[:], h_.ap[:], ALU.mult, [h_], [sq[c % 2]])

    def M3b(c):
        st = st3[c % 6]
        self.red(DVE, st.ap[:, 4:8], sq[c % 2].ap[:], ALU.add, [sq[c % 2]], [st])
        self.ts(DVE, st.ap[:, 0:4], st.ap[:, 0:4], 1.0 / 128, None, ALU.mult, None, [st], [st])
        self.tt(DVE, st.ap[:, 8:12], st.ap[:, 0:4], st.ap[:, 0:4], ALU.mult, [st], [st])
        self.stt(DVE, st.ap[:, 4:8], st.ap[:, 4:8], 1.0 / 128, st.ap[:, 8:12], ALU.mult, ALU.subtract, [st], [st])

    def M3c(c):
        st = st3[c % 6]
        self.act(st.ap[:, 4:8], st.ap[:, 4:8], AF.Ln, [st], [st], bias=self.eps_col.ap[:, 0:1])
        self.act(st.ap[:, 4:8], st.ap[:, 4:8], AF.Exp, [st], [st], scale=-0.5)

    def M3d(c):
        csl = slice(c * 128, (c + 1) * 128)
        self.ld(mo[c % 2].ap[:], moT_v[:, :, csl], [self.R['moT']], [mo[c % 2]])
        st = st3[c % 6]
        h_ = h32[c % 6]
        self.tt(DVE, h_.ap[:], h_.ap[:], st.ap[:, 0:4].unsqueeze(2).to_broadcast([128, 4, 128]), ALU.subtract,
                [h_, st], [h_])
        hb = hnb[c % 2]
        self.tt(DVE, hb.ap[:], h_.ap[:], st.ap[:, 4:8].unsqueeze(2).to_broadcast([128, 4, 128]), ALU.mult,
                [h_, st], [hb])

    def M4(c):
        csl = slice(c * 128, (c + 1) * 128)
        hb = hnb[c % 2]
        bH = self.bank_from('mH', [6])
        bHv = bH.ap[:].bitcast(BF16)
        for h in range(4):
            self.tr(bHv[:, h * 128:(h + 1) * 128], hb.ap[:, h, :], self.ident_bf.ap[:], [hb, self.ident_bf], [bH])
        yo = ymo[c % 2]
        for h in range(4):
            self.stt(DVE, yo.ap[:, h, :], bHv[:, h * 128:(h + 1) * 128], normw.ap[:, h:h + 1], mo[c % 2].ap[:, h, :],
                     ALU.mult, ALU.mult, [bH, normw, mo[c % 2]], [yo])
        self.ld(ymT_v[:, :, csl], yo.ap[:], [yo], [self.R['ymT']])

    def M01(c):
        M0(c)
        M1(c)

    self.pipeline_fine(NT, [M01, M2, M3a, M3b, M3c, M3d, M4])
    self.pop()


Prog.phase_mlstm = _phase_mlstm


def _bank_from(self, key, lst):
    cnt = self.__dict__.setdefault('_bank_cnt', {})
    i = cnt.get(key, 0)
    cnt[key] = i + 1
    return self.banks[lst[i % len(lst)]]


Prog.bank_from = _bank_from


def _pipeline(self, n, stages):
    ns = len(stages)
    for step in range(n + ns - 1):
        for si, stg in enumerate(stages):
            k = step - si
            if 0 <= k < n:
                stg(k)


Prog.pipeline = _pipeline


def _phase_fox(self, l):
    self.push()
    fw, nc = self.fw, self.nc
    DVE, ACT, POOL, PE = self.DVE, self.ACT, self.POOL, self.PE
    self.push()
    Ga = fw.sb("f_Ga", [8, S], F32)
    ON = fw.sb("f_ON", [8, S], F32)
    Fc = fw.sb("f_F", [8, S], F32)
    FH = fw.sb("f_FH", [8, S], BF16)
    FL = fw.sb("f_FL", [8, S], BF16)
    self.ld(Ga.ap[:], self.scr['gsm'][8:16, :], [self.R['gsm']], [Ga])
    self.act(Ga.ap[:], Ga.ap[:], AF.Exp, [Ga], [Ga], scale=-1.0)
    self.act(Ga.ap[:], Ga.ap[:], AF.Ln, [Ga], [Ga], bias=1.0)
    self.memset(DVE, ON.ap[:], 1.0, [ON])
    fw.op(DVE, lambda: nc.vector.tensor_tensor_scan(out=Fc.ap[:], data0=ON.ap[:], data1=Ga.ap[:], initial=0.0,
                                                    op0=ALU.mult, op1=ALU.subtract), [ON, Ga], [Fc])
    self.cp(DVE, FH.ap[:], Fc.ap[:], [Fc], [FH])
    self.tt(DVE, FL.ap[:], Fc.ap[:], FH.ap[:], ALU.subtract, [Fc, FH], [FL])
    self.ld(self.scr['fx'][0], FH.ap[:], [FH], [self.R['fx']])
    self.ld(self.scr['fx'][1], FL.ap[:], [FL], [self.R['fx']])
    NH = fw.sb("f_NH", [8, S], BF16)
    NL = fw.sb("f_NL", [8, S], BF16)
    self.ts(DVE, NH.ap[:], FH.ap[:], -1.0, None, ALU.mult, None, [FH], [NH])
    self.ts(DVE, NL.ap[:], FL.ap[:], -1.0, None, ALU.mult, None, [FL], [NL])
    self.ld(self.scr['fx'][2], NH.ap[:], [NH], [self.R['fx']])
    self.ld(self.scr['fx'][3], NL.ap[:], [NL], [self.R['fx']])
    self.pop()
    maskneg = fw.sb("f_mask", [128, 128], F32)
    self.ld(maskneg.ap[:], self.cst['c_maskneg'], [self.R_c], [maskneg])
    Vall = fw.sb("f_V", [128, NT, 8, 65], BF16)
    for h in range(8):
        self.ld(Vall.ap[:, :, h, 0:64], self.scr['va'][:, h * 64:(h + 1) * 64].rearrange("(nt p) c -> p nt c", p=128),
                [self.R['va']], [Vall])
    self.memset(POOL, Vall.ap[:, :, :, 64:65], 1.0, [Vall])
    Ytok = fw.sb("f_Y", [128, NT, 512], BF16)
    qa = [fw.sb("f_qa%d" % i, [68, S], BF16) for i in range(2)]
    ka = [fw.sb("f_ka%d" % i, [68, S], BF16) for i in range(2)]
    for i in range(2):
        self.memset(DVE, qa[i].ap[64:68, :], 1.0, [qa[i]])
        self.memset(DVE, ka[i].ap[64:68, :], 1.0, [ka[i]])
    Ssb = [fw.sb("f_S%d" % i, [128, S], F32) for i in range(4)]
    Pbf = [fw.sb("f_P%d" % i, [128, S], BF16) for i in range(3)]
    PT = [fw.sb("f_PT%d" % i, [128, 512], BF16) for i in range(4)]
    st = fw.sb("f_st", [128, 16], F32)
    fx = self.scr['fx']
    iters = [(h, qb) for h in range(8) for qb in range(NT)]
    state = {'pti': 0}

    def load_head(h):
        q_, k_ = qa[h % 2], ka[h % 2]
        self.ld(q_.ap[0:64, :], self.scr['aqT'][h * 64:(h + 1) * 64, :], [self.R['aqT']], [q_])
        self.ld(q_.ap[66:67, :], fx[0, h:h + 1, :], [self.R['fx']], [q_])
        self.ld(q_.ap[67:68, :], fx[1, h:h + 1, :], [self.R['fx']], [q_])
        self.ld(k_.ap[0:64, :], self.scr['akT'][h * 64:(h + 1) * 64, :], [self.R['akT']], [k_])
        self.ld(k_.ap[64:65, :], fx[2, h:h + 1, :], [self.R['fx']], [k_])
        self.ld(k_.ap[65:66, :], fx[3, h:h + 1, :], [self.R['fx']], [k_])

    def stA(k):
        h, qb = iters[k]
        if qb == 0:
            if h == 0:
                load_head(0)
            if h + 1 < 8:
                load_head(h + 1)
        q_, k_ = qa[h % 2], ka[h % 2]
        qsl = slice(qb * 128, (qb + 1) * 128)
        nk = (qb + 1) * 128
        S_ = Ssb[k % 4]
        for j in range((nk + 511) // 512):
            w = min(512, nk - j * 512)
            b = self.bank_from('fS', [0, 1, 2, 3])
            self.mm(b.ap[:, 0:w], q_.ap[0:68, qsl], k_.ap[0:68, j * 512:j * 512 + w], True, True, [q_, k_], [b])
            self.cp(ACT, S_.ap[:, j * 512:j * 512 + w], b.ap[:, 0:w], [b], [S_])

    def stB(k):
        h, qb = iters[k]
        qsl = slice(qb * 128, (qb + 1) * 128)
        nk = (qb + 1) * 128
        S_ = Ssb[k % 4]
        c0 = (k % 4) * 4
        self.tt(DVE, S_.ap[:, qsl], S_.ap[:, qsl], maskneg.ap[:], ALU.add, [S_, maskneg], [S_])
        self.red(DVE, st.ap[:, c0:c0 + 1], S_.ap[:, 0:nk], ALU.max, [S_], [st])
        self.ts(DVE, st.ap[:, c0 + 1:c0 + 2], st.ap[:, c0:c0 + 1], -1.0, None, ALU.mult, None, [st], [st])

    def stC(k):
        h, qb = iters[k]
        nk = (qb + 1) * 128
        c0 = (k % 4) * 4
        self.act(Pbf[k % 3].ap[:, 0:nk], Ssb[k % 4].ap[:, 0:nk], AF.Exp, [Ssb[k % 4], st], [Pbf[k % 3]],
                 bias=st.ap[:, c0 + 1:c0 + 2])

    def stD(k):
        h, qb = iters[k]
        P_ = Pbf[k % 3]
        c0 = (k % 4) * 4
        bO = self.bank_from('fO', [6, 7])
        groups = [list(range(g * 4, min(g * 4 + 4, qb + 1))) for g in range((qb + 4) // 4)]
        pts = []

        def do_tr(kbs):
            bT = self.bank_from('fT', [4, 5])
            bTv = bT.ap[:].bitcast(BF16)
            for i, kb in enumerate(kbs):
                self.tr(bTv[:, i * 128:(i + 1) * 128], P_.ap[:, kb * 128:(kb + 1) * 128], self.ident_bf.ap[:],
                        [P_, self.ident_bf], [bT])
            pt = PT[state['pti'] % 4]
            state['pti'] += 1
            self.cp(DVE, pt.ap[:, 0:len(kbs) * 128], bTv[:, 0:len(kbs) * 128], [bT], [pt])
            return pt

        def do_pv(kbs, pt):
            for i, kb in enumerate(kbs):
                self.mm(bO.ap[:, 0:65], pt.ap[:, i * 128:(i + 1) * 128], Vall.ap[:, kb, h, :], kb == 0, kb == qb,
                        [pt, Vall], [bO])
        prev = None
        for kbs in groups:
            pt = do_tr(kbs)
            if prev is not None:
                do_pv(*prev)
            prev = (kbs, pt)
        do_pv(*prev)
        fw.op(DVE, lambda: nc.vector.reciprocal(out=st.ap[:, c0 + 2:c0 + 3], in_=bO.ap[:, 64:65]), [bO], [st])
        self.ts(DVE, Ytok.ap[:, qb, h * 64:(h + 1) * 64], bO.ap[:, 0:64], st.ap[:, c0 + 2:c0 + 3], None, ALU.mult, None,
                [bO, st], [Ytok])

    self.pipeline(len(iters), [stA, stB, stC, stD])
    yo = [fw.sb("f_yo%d" % i, [128, 4, 128], BF16) for i in range(2)]
    yaT_v = self.scr['yaT'].rearrange("(f p) t -> p f t", p=128)
    for qb in range(NT):
        bT = self.bank_from('fT', [4, 5])
        bTv = bT.ap[:].bitcast(BF16)
        for f in range(4):
            self.tr(bTv[:, f * 128:(f + 1) * 128], Ytok.ap[:, qb, f * 128:(f + 1) * 128], self.ident_bf.ap[:],
                    [Ytok, self.ident_bf], [bT])
        y_ = yo[qb % 2]
        self.cp(ACT, y_.ap[:].rearrange("p f t -> p (f t)"), bTv[:, 0:512], [bT], [y_])
        self.ld(yaT_v[:, :, qb * 128:(qb + 1) * 128], y_.ap[:], [y_], [self.R['yaT']])
    self.pop()


Prog.phase_fox = _phase_fox


def _phase_s5(self, l):
    self.push()
    fw, nc = self.fw, self.nc
    DVE, ACT, POOL, PE = self.DVE, self.ACT, self.POOL, self.PE
    inp = self.inp
    L1 = fw.sb("s_L1", [128, 32, 128], BF16)
    L2 = fw.sb("s_L2", [128, 32, 128], BF16)
    W1 = fw.sb("s_W1", [128, 512], BF16)
    W2 = fw.sb("s_W2", [128, 512], BF16)
    Dsel = fw.sb("s_Dsel", [128, 32, 16], BF16)
    thp = fw.sb("s_thp", [128, 32], F32)
    RP = fw.sb("s_RP", [128, 32], F32)
    thb = fw.sb("s_thb", [128, 32], F32)
    self.push()
    LR = fw.sb("s_LR", [128, 32], F32)
    LI = fw.sb("s_LI", [128, 32], F32)
    DT = fw.sb("s_DT", [128, 32], F32)
    for half in range(2):
        ps = slice(half * 64, half * 64 + 64)
        self.ld(LR.ap[ps, :], inp['s5_lam_re'][l].rearrange("g p -> p g"), [self.R_w], [LR], allow_slow_non_contiguous=True)
        self.ld(LI.ap[ps, :], inp['s5_lam_im'][l].rearrange("g p -> p g"), [self.R_w], [LI], allow_slow_non_contiguous=True)
    self.ld(DT.ap[:], inp['s5_log_dt'][l].partition_broadcast(128), [self.R_w], [DT])
    self.act(DT.ap[:], DT.ap[:], AF.Exp, [DT], [DT])
    t = [fw.sb("s_t%d" % i, [128, 32], F32) for i in range(12)]
    ti = fw.sb("s_ti", [128, 32], I32)
    TH, ST1, CT1, AR, AI, DEN, ZR, ZI, ZIs, ZRs2, tmpa, tmpb = t
    self.tt(DVE, TH.ap[:], LI.ap[:], DT.ap[:], ALU.mult, [LI, DT], [TH])
    self.ts(DVE, thp.ap[:], TH.ap[:], 1.0 / TWO_PI, None, ALU.mult, None, [TH], [thp])
    self.ts(DVE, thb.ap[:], thp.ap[:], 2048.0, None, ALU.mult, None, [thp], [thb])
    self.tt(DVE, tmpa.ap[:], LR.ap[:], DT.ap[:], ALU.mult, [LR, DT], [tmpa])
    self.act(RP.ap[:], tmpa.ap[:], AF.Exp, [tmpa], [RP])
    self.cp(DVE, ti.ap[:], thp.ap[:], [thp], [ti])
    self.tt(DVE, tmpa.ap[:], thp.ap[:], ti.ap[:], ALU.subtract, [thp, ti], [tmpa])
    self.act(ST1.ap[:], tmpa.ap[:], AF.Sin, [tmpa], [ST1], scale=TWO_PI)
    self.ts(DVE, tmpb.ap[:], thp.ap[:], 0.25, None, ALU.add, None, [thp], [tmpb])
    self.cp(DVE, ti.ap[:], tmpb.ap[:], [tmpb], [ti])
    self.tt(DVE, tmpb.ap[:], tmpb.ap[:], ti.ap[:], ALU.subtract, [tmpb, ti], [tmpb])
    self.act(CT1.ap[:], tmpb.ap[:], AF.Sin, [tmpb], [CT1], scale=TWO_PI)
    self.tt(DVE, AR.ap[:], RP.ap[:], CT1.ap[:], ALU.mult, [RP, CT1], [AR])
    self.tt(DVE, AI.ap[:], RP.ap[:], ST1.ap[:], ALU.mult, [RP, ST1], [AI])
    self.ts(DVE, AR.ap[:], AR.ap[:], -1.0, None, ALU.add, None, [AR], [AR])
    self.tt(DVE, DEN.ap[:], LR.ap[:], LR.ap[:], ALU.mult, [LR], [DEN])
    self.tt(DVE, tmpa.ap[:], LI.ap[:], LI.ap[:], ALU.mult, [LI], [tmpa])
    self.tt(DVE, DEN.ap[:], DEN.ap[:], tmpa.ap[:], ALU.add, [DEN, tmpa], [DEN])
    fw.op(DVE, lambda: nc.vector.reciprocal(out=DEN.ap[:], in_=DEN.ap[:]), [DEN], [DEN])
    self.tt(DVE, tmpa.ap[:], AR.ap[:], LR.ap[:], ALU.mult, [AR, LR], [tmpa])
    self.tt(DVE, tmpb.ap[:], AI.ap[:], LI.ap[:], ALU.mult, [AI, LI], [tmpb])
    self.tt(DVE, tmpa.ap[:], tmpa.ap[:], tmpb.ap[:], ALU.add, [tmpa, tmpb], [tmpa])
    self.tt(DVE, ZR.ap[:], tmpa.ap[:], DEN.ap[:], ALU.mult, [tmpa, DEN], [ZR])
    self.tt(DVE, tmpa.ap[:], AI.ap[:], LR.ap[:], ALU.mult, [AI, LR], [tmpa])
    self.tt(DVE, tmpb.ap[:], AR.ap[:], LI.ap[:], ALU.mult, [AR, LI], [tmpb])
    self.tt(DVE, tmpa.ap[:], tmpa.ap[:], tmpb.ap[:], ALU.subtract, [tmpa, tmpb], [tmpa])
    self.tt(DVE, ZI.ap[:], tmpa.ap[:], DEN.ap[:], ALU.mult, [tmpa, DEN], [ZI])
    self.cp(DVE, ZIs.ap[:], ZI.ap[:], [ZI], [ZIs])
    self.ts(DVE, ZIs.ap[0:64, :], ZI.ap[0:64, :], -1.0, None, ALU.mult, None, [ZI], [ZIs])
    self.cp(DVE, ZRs2.ap[:], ZR.ap[:], [ZR], [ZRs2])
    self.ts(DVE, ZRs2.ap[64:128, :], ZR.ap[64:128, :], -1.0, None, ALU.mult, None, [ZR], [ZRs2])
    Bst = fw.sb("s_Bst", [128, 32, 16], F32)
    Bsw = fw.sb("s_Bsw", [128, 32, 16], F32)
    bre = inp['s5_b_re'][l].rearrange("g p c -> p g c")
    bim = inp['s5_b_im'][l].rearrange("g p c -> p g c")
    self.ld(Bst.ap[0:64], bre, [self.R_w], [Bst])
    self.ld(Bst.ap[64:128], bim, [self.R_w], [Bst])
    self.ld(Bsw.ap[0:64], bim, [self.R_w], [Bsw])
    self.ld(Bsw.ap[64:128], bre, [self.R_w], [Bsw])
    M1 = fw.sb("s_M1", [128, 32, 16], F32)
    M2 = fw.sb("s_M2", [128, 32, 16], F32)
    Mt = fw.sb("s_Mt", [128, 32, 16], F32)

    def bc(z):
        return z.ap[:].unsqueeze(2).to_broadcast([128, 32, 16])
    self.tt(DVE, M1.ap[:], Bst.ap[:], bc(ZR), ALU.mult, [Bst, ZR], [M1])
    self.tt(DVE, Mt.ap[:], Bsw.ap[:], bc(ZIs), ALU.mult, [Bsw, ZIs], [Mt])
    self.tt(DVE, M1.ap[:], M1.ap[:], Mt.ap[:], ALU.add, [M1, Mt], [M1])
    self.tt(DVE, M2.ap[:], Bsw.ap[:], bc(ZRs2), ALU.mult, [Bsw, ZRs2], [M2])
    self.tt(DVE, Mt.ap[:], Bst.ap[:], bc(ZI), ALU.mult, [Bst, ZI], [Mt])
    self.tt(DVE, M2.ap[:], M2.ap[:], Mt.ap[:], ALU.add, [M2, Mt], [M2])
    Mpad = fw.sb("s_Mpad", [128, 32, 128], F32)
    for (M, Lx) in ((M1, L1), (M2, L2)):
        self.memset(POOL, Mpad.ap[:], 0.0, [Mpad])
        for gl in range(8):
            self.cp(DVE, Mpad.ap[:].rearrange("m (ft gl) k -> m ft gl k", gl=8)[:, :, gl, gl * 16:(gl + 1) * 16],
                    M.ap[:].rearrange("m (ft gl) c -> m ft gl c", gl=8)[:, :, gl, :], [M], [Mpad])
        for g4 in range(8):
            b = self.bank()
            for i in range(4):
                g = g4 * 4 + i
                self.tr(b.ap[:, i * 128:(i + 1) * 128], Mpad.ap[:, g, :], self.ident_f.ap[:], [Mpad, self.ident_f], [b])
            self.cp(ACT, Lx.ap[:, g4 * 4:(g4 + 1) * 4, :].rearrange("k g m -> k (g m)"), b.ap[:], [b], [Lx])
    CC = fw.sb("s_CC", [128, 4, 128], F32)
    CC2 = fw.sb("s_CC2", [128, 4, 128], F32)
    cre = inp['s5_c_re'][l].rearrange("(ft gl) c p -> (gl c) ft p", gl=8)
    cim = inp['s5_c_im'][l].rearrange("(ft gl) c p -> (gl c) ft p", gl=8)
    self.ld(CC.ap[:, :, 0:64], cre, [self.R_w], [CC])
    self.ld(CC.ap[:, :, 64:128], cim, [self.R_w], [CC])
    self.ld(CC2.ap[:, :, 0:64], cim, [self.R_w], [CC2])
    self.ld(CC2.ap[:, :, 64:128], cre, [self.R_w], [CC2])
    b = self.bank()
    for ft in range(4):
        self.tr(b.ap[:, ft * 128:(ft + 1) * 128], CC.ap[:, ft, :], self.ident_f.ap[:], [CC, self.ident_f], [b])
    self.cp(DVE, W1.ap[0:64, :], b.ap[0:64, :], [b], [W1])
    self.ts(DVE, W1.ap[64:128, :], b.ap[64:128, :], -1.0, None, ALU.mult, None, [b], [W1])
    b = self.bank()
    for ft in range(4):
        self.tr(b.ap[:, ft * 128:(ft + 1) * 128], CC2.ap[:, ft, :], self.ident_f.ap[:], [CC2, self.ident_f], [b])
    self.ts(DVE, W2.ap[:], b.ap[:], -1.0, None, ALU.mult, None, [b], [W2])
    esel = fw.sb("s_esel", [128, 8, 16], F32)
    dcol = fw.sb("s_dcol", [128, 4], F32)
    self.ld(esel.ap[:], self.cst['c_esel'], [self.R_c], [esel])
    self.ld(dcol.ap[:], inp['s5_d'][l].rearrange("(ft gl) c -> (gl c) ft", gl=8), [self.R_w], [dcol],
            allow_slow_non_contiguous=True)
    for ft in range(4):
        self.ts(DVE, Dsel.ap[:, ft * 8:(ft + 1) * 8, :], esel.ap[:], dcol.ap[:, ft:ft + 1], None, ALU.mult, None,
                [esel, dcol], [Dsel])
    self.pop()
    self.push()
    HS = S // 2
    iota = fw.sb("s_iota", [128, HS], F32)
    self.ld(iota.ap[:], self.cst['c_iota'][:, 0:HS], [self.R_c], [iota])
    zcol = fw.sb("s_zcol", [128, 1], F32)
    self.memset(DVE, zcol.ap[:], 0.0, [zcol])
    hpi = fw.sb("s_hpi", [128, 1], F32)
    self.memset(DVE, hpi.ap[:], TWO_PI / 4.0, [hpi])
    PH = [fw.sb("s_PH%d" % i, [128, HS], F32) for i in range(3)]
    PI = [fw.sb("s_PI%d" % i, [128, HS], I32) for i in range(2)]
    SN = [fw.sb("s_SN%d" % i, [128, HS], F32) for i in range(3)]
    CSt = [fw.sb("s_CS%d" % i, [128, HS], F32) for i in range(3)]
    XP = [fw.sb("s_XP%d" % i, [128, HS], F32) for i in range(2)]
    Gt = [fw.sb("s_G%d" % i, [128, HS], F32) for i in range(2)]
    Gc = [fw.sb("s_Gc%d" % i, [128, HS], BF16) for i in range(2)]
    Gs = [fw.sb("s_Gs%d" % i, [128, HS], BF16) for i in range(2)]
    uT = [fw.sb("s_uT%d" % i, [128, S], BF16) for i in range(2)]
    ysb = [fw.sb("s_ysb%d" % i, [16, HS], F32) for i in range(2)]
    tmp = [fw.sb("s_tmp%d" % i, [128, 512], F32) for i in range(2)]
    cnt = {'t': 0}

    def S0a(k):
        g, hf = k // 2, k % 2
        ft = g // 8
        if g % 8 == 0 and hf == 0:
            self.ld(uT[ft % 2].ap[:], self.scr['suT'][ft * 128:(ft + 1) * 128, :], [self.R['suT']], [uT[ft % 2]])
        ph, pi = PH[k % 3], PI[k % 2]
        bias = thb.ap[:, g:g + 1] if hf else zcol.ap[:, 0:1]
        self.act(ph.ap[:], iota.ap[:], AF.Identity, [iota, thp, thb, zcol], [ph], bias=bias, scale=thp.ap[:, g:g + 1])
        self.act(pi.ap[:], iota.ap[:], AF.Identity, [iota, thp, thb, zcol], [pi], bias=bias, scale=thp.ap[:, g:g + 1])

    def S0b(k):
        ph, pi = PH[k % 3], PI[k % 2]
        self.tt(DVE, ph.ap[:], ph.ap[:], pi.ap[:], ALU.subtract, [ph, pi], [ph])

    def S0c(k):
        ph = PH[k % 3]
        self.act(SN[k % 3].ap[:], ph.ap[:], AF.Sin, [ph], [SN[k % 3]], scale=TWO_PI)
        self.act(ph.ap[:], ph.ap[:], AF.Abs, [ph], [ph])
        self.act(CSt[k % 3].ap[:], ph.ap[:], AF.Sin, [ph, hpi], [CSt[k % 3]], scale=-TWO_PI, bias=hpi.ap[:, 0:1])

    def S1(k):
        g, hf = k // 2, k % 2
        u_ = uT[(g // 8) % 2]
        sn, cs, xp = SN[k % 3], CSt[k % 3], XP[k % 2]
        for j in range(4):
            sl = slice(j * 512, (j + 1) * 512)
            usl = slice(hf * HS + j * 512, hf * HS + (j + 1) * 512)
            b1 = self.bank_from('sX', [0, 1, 2, 3])
            self.mm(b1.ap[:], L1.ap[:, g, :], u_.ap[:, usl], True, True, [L1, u_], [b1])
            b2 = self.bank_from('sX', [0, 1, 2, 3])
            self.mm(b2.ap[:], L2.ap[:, g, :], u_.ap[:, usl], True, True, [L2, u_], [b2])
            tm = tmp[cnt['t'] % 2]
            cnt['t'] += 1
            self.tt(DVE, xp.ap[:, sl], cs.ap[:, sl], b1.ap[:], ALU.mult, [cs, b1], [xp])
            self.tt(DVE, tm.ap[:], sn.ap[:, sl], b2.ap[:], ALU.mult, [sn, b2], [tm])
            self.tt(POOL, xp.ap[:, sl], xp.ap[:, sl], tm.ap[:], ALU.add, [xp, tm], [xp])

    def S2(k):
        g, hf = k // 2, k % 2
        sn, cs, xp, g_ = SN[k % 3], CSt[k % 3], XP[k % 2], Gt[k % 2]
        if hf == 0:
            fw.op(DVE, lambda: nc.vector.tensor_tensor_scan(out=g_.ap[:], data0=RP.ap[:, g:g + 1].to_broadcast([128, HS]),
                                                            data1=xp.ap[:], initial=0.0, op0=ALU.mult, op1=ALU.add),
                  [RP, xp], [g_])
        else:
            gp = Gt[(k - 1) % 2]
            fw.op(DVE, lambda: nc.vector.tensor_tensor_scan(out=g_.ap[:], data0=RP.ap[:, g:g + 1].to_broadcast([128, HS]),
                                                            data1=xp.ap[:], initial=gp.ap[:, HS - 1:HS],
                                                            op0=ALU.mult, op1=ALU.add),
                  [RP, xp, gp], [g_])
        self.tt(DVE, Gc[k % 2].ap[:], cs.ap[:], g_.ap[:], ALU.mult, [cs, g_], [Gc[k % 2]])
        self.tt(POOL, Gs[k % 2].ap[:], sn.ap[:], g_.ap[:], ALU.mult, [sn, g_], [Gs[k % 2]])

    def S3(k):
        g, hf = k // 2, k % 2
        u_ = uT[(g // 8) % 2]
        y_ = ysb[k % 2]
        for j in range(4):
            sl = slice(j * 512, (j + 1) * 512)
            usl = slice(hf * HS + j * 512, hf * HS + (j + 1) * 512)
            b = self.bank_from('sY', [4, 5, 6, 7])
            self.mm(b.ap[0:16, :], W1.ap[:, g * 16:(g + 1) * 16], Gc[k % 2].ap[:, sl], True, False, [W1, Gc[k % 2]], [b])
            self.mm(b.ap[0:16, :], W2.ap[:, g * 16:(g + 1) * 16], Gs[k % 2].ap[:, sl], False, False, [W2, Gs[k % 2]], [b])
            self.mm(b.ap[0:16, :], Dsel.ap[:, g, :], u_.ap[:, usl], False, True, [Dsel, u_], [b])
            self.cp(ACT, y_.ap[:, sl], b.ap[0:16, :], [b], [y_])
        self.ld(self.scr['s5y'][g * 16:(g + 1) * 16, hf * HS:(hf + 1) * HS], y_.ap[:], [y_], [self.R['s5y']])

    self.pipeline_fine(64, [S0a, S0b, S0c, S1, S2, S3])
    self.pop()
    self.push()
    wgb = fw.sb("g_wgb", [128, 4, 512], BF16)
    self.ld(wgb.ap[:], inp['s5_w_glu'][l].rearrange("(kt p) c -> p kt c", p=128), [self.R_w], [wgb], q=POOL)
    bg = fw.sb("g_bg", [128, 4], F32)
    self.ld(bg.ap[:], inp['s5_b_glu'][l].rearrange("(t p) -> p t", p=128), [self.R_w], [bg], allow_slow_non_contiguous=True)
    xs = [fw.sb("g_xs%d" % i, [128, 4, 512], F32) for i in range(3)]
    x2 = [fw.sb("g_x2%d" % i, [128, 4, 512], F32) for i in range(2)]
    ys = [fw.sb("g_ys%d" % i, [128, 4, 512], F32) for i in range(2)]
    yb = [fw.sb("g_yb%d" % i, [128, 4, 512], BF16) for i in range(2)]
    sg = [fw.sb("g_sg%d" % i, [128, 512], F32) for i in range(2)]
    yo = [fw.sb("g_yo%d" % i, [128, 4, 512], BF16) for i in range(2)]
    s5v = self.scr['s5y'].rearrange("(f p) t -> p f t", p=128)
    ysv = self.scr['ysT'].rearrange("(f p) t -> p f t", p=128)
    GC = 0.7978845608028654

    def GA(tc):
        sl = slice(tc * 512, (tc + 1) * 512)
        self.ld(xs[tc % 3].ap[:], s5v[:, :, sl], [self.R['s5y']], [xs[tc % 3]])

    def GB(tc):
        x_, x2_, y_, yb_ = xs[tc % 3], x2[tc % 2], ys[tc % 2], yb[tc % 2]
        self.tt(POOL, x2_.ap[:], x_.ap[:], x_.ap[:], ALU.mult, [x_], [x2_])
        self.ts(DVE, x2_.ap[:], x2_.ap[:], 0.044715, 1.0, ALU.mult, ALU.add, [x2_], [x2_])
        self.tt(DVE, x2_.ap[:], x2_.ap[:], x_.ap[:], ALU.mult, [x2_, x_], [x2_])
        self.act(x2_.ap[:], x2_.ap[:], AF.Sigmoid, [x2_], [x2_], scale=2.0 * GC)
        self.tt(DVE, y_.ap[:], x_.ap[:], x2_.ap[:], ALU.mult, [x_, x2_], [y_])
        self.cp(POOL, yb_.ap[:], y_.ap[:], [y_], [yb_])

    def GD(tc):
        sl = slice(tc * 512, (tc + 1) * 512)
        y_, yb_, yo_ = ys[tc % 2], yb[tc % 2], yo[tc % 2]
        for f in range(4):
            b = self.bank()
            for kt in range(4):
                self.mm(b.ap[:], wgb.ap[:, kt, f * 128:(f + 1) * 128], yb_.ap[:, kt, :], kt == 0, kt == 3, [wgb, yb_], [b])
            sg_ = sg[f % 2]
            self.act(sg_.ap[:], b.ap[:], AF.Sigmoid, [b, bg], [sg_], bias=bg.ap[:, f:f + 1])
            self.tt(DVE, yo_.ap[:, f, :], y_.ap[:, f, :], sg_.ap[:], ALU.mult, [y_, sg_], [yo_])
        self.ld(ysv[:, :, sl], yo_.ap[:], [yo_], [self.R['ysT']])

    self.pipeline_fine(NCH, [GA, GB, GD])
    self.pop()
    self.pop()


Prog.phase_s5 = _phase_s5


def _ln_stats(self, r, st, c0, junk):
    self.act(junk.ap[:], r.ap[:], AF.Identity, [r], [junk, st], accum=st.ap[:, c0 + 0:c0 + 1])
    self.act(junk.ap[:], r.ap[:], AF.Square, [r], [junk, st], accum=st.ap[:, c0 + 1:c0 + 2])


def _ln_rstd(self, st, c0):
    DVE = self.DVE
    self.ts(DVE, st.ap[:, c0 + 2:c0 + 3], st.ap[:, c0 + 0:c0 + 1], 1.0 / D, None, ALU.mult, None, [st], [st])
    self.tt(DVE, st.ap[:, c0 + 3:c0 + 4], st.ap[:, c0 + 2:c0 + 3], st.ap[:, c0 + 2:c0 + 3], ALU.mult, [st], [st])
    self.stt(DVE, st.ap[:, c0 + 4:c0 + 5], st.ap[:, c0 + 1:c0 + 2], 1.0 / D, st.ap[:, c0 + 3:c0 + 4], ALU.mult, ALU.subtract,
             [st], [st])
    self.act(st.ap[:, c0 + 4:c0 + 5], st.ap[:, c0 + 4:c0 + 5], AF.Ln, [st], [st], bias=self.eps_col.ap[:, 0:1])
    self.act(st.ap[:, c0 + 5:c0 + 6], st.ap[:, c0 + 4:c0 + 5], AF.Exp, [st], [st], scale=-0.5)


def _ln_apply(self, r, st, c0, g_bc, b_bc, xo, xb=None):
    DVE, POOL = self.DVE, self.POOL
    self.ts(DVE, r.ap[:], r.ap[:], st.ap[:, c0 + 2:c0 + 3], st.ap[:, c0 + 5:c0 + 6], ALU.subtract, ALU.mult, [r, st], [r])
    self.tt(DVE, r.ap[:], r.ap[:], g_bc.ap[:], ALU.mult, [r, g_bc], [r])
    self.tt(DVE, xo.ap[:], r.ap[:], b_bc.ap[:], ALU.add, [r, b_bc], [xo])
    if xb is not None:
        self.cp(self.ACT, xb.ap[:], xo.ap[:], [xo], [xb])


Prog.ln_stats = _ln_stats
Prog.ln_rstd = _ln_rstd
Prog.ln_apply = _ln_apply


def _layer_norm(self, r, g_bc, b_bc, st, junk, xo, xb=None):
    self.ln_stats(r, st, 0, junk)
    self.ln_rstd(st, 0)
    self.ln_apply(r, st, 0, g_bc, b_bc, xo, xb)


def _pipeline_steps(self, n, stages):
    ns = len(stages)
    steps = []
    for step in range(n + ns - 1):
        def f(step=step):
            for si, stg in enumerate(stages):
                k = step - si
                if 0 <= k < n:
                    stg(k)
        steps.append(f)
    return steps


def _interleave(self, *lists):
    m = max(len(x) for x in lists)
    for i in range(m):
        for x in lists:
            if i < len(x):
                x[i]()


Prog.pipeline_steps = _pipeline_steps


def _pipeline_fine(self, n, stages):
    ns = len(stages)
    fw = self.fw
    for step in range(n + ns - 1):
        lists = []
        for si, stg in enumerate(stages):
            k = step - si
            if 0 <= k < n:
                fw._defer = []
                stg(k)
                lists.append(fw._defer)
                fw._defer = None
        m = max(len(x) for x in lists)
        for i in range(m):
            for x in lists:
                if i < len(x):
                    f, a, kw = x[i]
                    f(*a, **kw)


Prog.pipeline_fine = _pipeline_fine
Prog.interleave = _interleave


Prog.layer_norm = _layer_norm


def _phase_merge(self, l):
    self.push()
    fw, nc = self.fw, self.nc
    DVE, ACT, POOL, PE = self.DVE, self.ACT, self.POOL, self.PE
    inp = self.inp
    wbr = fw.sb("g_wbr", [128, 3, 4, 1024], BF16)
    wout = fw.sb("g_wout", [128, 8, 1024], BF16)
    for b in range(3):
        self.ld(wbr.ap[:, b], inp['w_branch'][l, b].rearrange("(kt p) c -> p kt c", p=128), [self.R_w], [wbr], q=POOL)
    for hf in range(2):
        self.ld(wout.ap[:, hf * 4:(hf + 1) * 4, :],
                inp['w_out'][l, hf * 512:(hf + 1) * 512, :].rearrange("(kt p) c -> p kt c", p=128),
                [self.R_w], [wout], q=POOL)
    g_bc = fw.sb("g_gbc", [128, D], F32)
    b_bc = fw.sb("g_bbc", [128, D], F32)
    self.ld(g_bc.ap[:], inp['ln1_g'][l].partition_broadcast(128), [self.R_w], [g_bc])
    self.ld(b_bc.ap[:], inp['ln1_b'][l].partition_broadcast(128), [self.R_w], [b_bc])
    yt = [[fw.sb("g_y%d_%d" % (b, i), [128, 4, 512], BF16) for b in range(3)] for i in range(2)]
    mixT = [fw.sb("g_mix%d" % i, [128, 8, 512], BF16) for i in range(2)]
    gt = [fw.sb("g_gt%d" % i, [128, 3, 512], F32) for i in range(3)]
    mt = [[fw.sb("g_mt%d_%d" % (b, i), [128, 512], F32) for b in range(3)] for i in range(2)]
    xt = [fw.sb("g_xt%d" % i, [128, D], F32) for i in range(2)]
    rt = [fw.sb("g_rt%d" % i, [128, D], F32) for i in range(4)]
    xo = [fw.sb("g_xo%d" % i, [128, D], F32) for i in range(2)]
    xb = [fw.sb("g_xb%d" % i, [128, D], BF16) for i in range(2)]
    stg = [fw.sb("g_stg%d" % i, [128, D], BF16) for i in range(2)]
    junk = fw.sb("g_junk", [128, D], F32)
    st = fw.sb("g_st", [128, 32], F32)
    ysrc = [self.scr[n].rearrange("(kt p) t -> p kt t", p=128) for n in ('ymT', 'ysT', 'yaT')]
    yres = [self.R[n] for n in ('ymT', 'ysT', 'yaT')]
    gv = self.scr['gT'].rearrange("(b f p) t -> p b f t", b=3, p=128)
    pbs = {}

    def br_steps(tc):
        sl = slice(tc * 512, (tc + 1) * 512)
        ys_ = yt[tc % 2]
        mx = mixT[tc % 2]

        def G0(ft):
            if ft == 0:
                for b in range(3):
                    self.ld(ys_[b].ap[:], ysrc[b][:, :, sl], [yres[b]], [ys_[b]])
            gi = tc * 8 + ft
            g_ = gt[gi % 3]
            self.ld(g_.ap[:], gv[:, :, ft, sl], [self.R['gT']], [g_])
            for b in range(3):
                pb = self.bank_from('mB', [0, 1, 2, 3, 4, 5])
                pbs[(gi, b)] = pb
                for kt in range(4):
                    self.mm(pb.ap[:], wbr.ap[:, b, kt, ft * 128:(ft + 1) * 128], ys_[b].ap[:, kt, :], kt == 0, kt == 3,
                            [wbr, ys_[b]], [pb])

        def G1(ft):
            gi = tc * 8 + ft
            g_ = gt[gi % 3]
            for b in range(3):
                pb = pbs.pop((gi, b))
                self.tt(DVE, mt[gi % 2][b].ap[:], pb.ap[:], g_.ap[:, b, :], ALU.mult, [pb, g_], [mt[gi % 2][b]])

        def G2(ft):
            gi = tc * 8 + ft
            m_ = mt[gi % 2]
            self.tt(DVE, m_[0].ap[:], m_[0].ap[:], m_[1].ap[:], ALU.add, [m_[0], m_[1]], [m_[0]])
            self.tt(DVE, mx.ap[:, ft, :], m_[0].ap[:], m_[2].ap[:], ALU.add, [m_[0], m_[2]], [mx])
        return self.pipeline_steps(8, [G0, G1, G2])

    def ol_steps(tc):
        mx = mixT[tc % 2]

        def T0(tq):
            i = tc * 4 + tq
            x_, r_ = xt[i % 2], rt[i % 4]
            self.ld(x_.ap[:], self.scr['xres0'][i * 128:(i + 1) * 128, :], [self.R['xres0']], [x_])
            for hf in range(2):
                pb = self.bank_from('mO', [6, 7])
                for kt in range(8):
                    self.mm(pb.ap[:], mx.ap[:, kt, tq * 128:(tq + 1) * 128], wout.ap[:, kt, hf * 512:(hf + 1) * 512],
                            kt == 0, kt == 7, [mx, wout], [pb])
                self.stt(DVE, r_.ap[:, hf * 512:(hf + 1) * 512], x_.ap[:, hf * 512:(hf + 1) * 512], ALPHA, pb.ap[:],
                         ALU.mult, ALU.add, [x_, pb], [r_])

        def T1(tq):
            i = tc * 4 + tq
            self.ln_stats(rt[i % 4], st, (i % 4) * 8, junk)

        def T2(tq):
            i = tc * 4 + tq
            self.ln_rstd(st, (i % 4) * 8)

        def T3(tq):
            i = tc * 4 + tq
            self.ln_apply(rt[i % 4], st, (i % 4) * 8, g_bc, b_bc, xo[i % 2], xb[i % 2])

        def T4(tq):
            i = tc * 4 + tq
            self.ld(self.scr['xres1'][i * 128:(i + 1) * 128, :], xo[i % 2].ap[:], [xo[i % 2]], [self.R['xres1']], q=POOL)
            self.ld(self.scr['x1b'][i * 128:(i + 1) * 128, :], xb[i % 2].ap[:], [xb[i % 2]], [self.R['x1b']], q=POOL)
            self.transpose_store(xb[i % 2].ap, xb[i % 2], self.scr['x1T'], self.R['x1T'], i, stg, i, pool=('mO', [6, 7]))
        return self.pipeline_steps(4, [T0, T1, T2, T3, T4])

    self.interleave(br_steps(0))
    for tc in range(NCH):
        nxt = br_steps(tc + 1) if tc + 1 < NCH else []
        self.interleave(nxt, ol_steps(tc))
    self.pop()


Prog.phase_merge = _phase_merge


def _phase_route(self, l):
    fw, nc = self.fw, self.nc
    DVE, ACT, POOL, PE = self.DVE, self.ACT, self.POOL, self.PE
    inp = self.inp
    DSTi, WT = self.DSTi, self.WT
    self.push()
    wr = fw.sb("r_wr", [128, 8, 36], BF16)
    self.ld(wr.ap[:, :, 0:4], inp['w_route_group'][l].rearrange("(kt p) c -> p kt c", p=128), [self.R_w], [wr], q=POOL)
    self.ld(wr.ap[:, :, 4:36], inp['w_route_expert'][l].rearrange("(kt p) c -> p kt c", p=128), [self.R_w], [wr], q=POOL)
    brt = fw.sb("r_brt", [128, 36], F32)
    self.ld(brt.ap[:, 0:4], inp['b_route_group'][l].partition_broadcast(128), [self.R_w], [brt])
    self.ld(brt.ap[:, 4:36], inp['b_route_expert'][l].partition_broadcast(128), [self.R_w], [brt])
    ustr = fw.sb("r_ustr", [128, 128], BF16)
    self.ld(ustr.ap[:], self.cst['c_ustrict'], [self.R_c], [ustr])
    ecap = fw.sb("r_ecap", [128, NE], F32)
    self.ld(ecap.ap[:], self.cst['c_ecap'], [self.R_c], [ecap])
    Asum = fw.sb("r_Asum", [128, NE], F32)
    Asb = fw.sb("r_Asb", [128, NE], BF16)
    self.memset(DVE, Asum.ap[:], 0.0, [Asum])
    self.memset(DVE, Asb.ap[:], 0.0, [Asb])
    xT_ = [fw.sb("r_xT%d" % i, [128, 8, 128], BF16) for i in range(2)]
    xb_ = [fw.sb("r_xb%d" % i, [128, D], BF16) for i in range(3)]
    lg_ = [fw.sb("r_lg%d" % i, [128, 36], F32) for i in range(2)]
    w_ = [fw.sb("r_w%d" % i, [128, 32], F32) for i in range(2)]
    w2_ = [fw.sb("r_w2%d" % i, [128, 8], F32) for i in range(2)]
    top8_ = [fw.sb("r_top8%d" % i, [128, 8], F32) for i in range(2)]
    A1_ = [fw.sb("r_A1%d" % i, [128, NE], F32) for i in range(2)]
    A2_ = [fw.sb("r_A2%d" % i, [128, NE], F32) for i in range(2)]
    A_ = [fw.sb("r_A%d" % i, [128, NE], F32) for i in range(2)]
    Ab_ = [fw.sb("r_Ab%d" % i, [128, NE], BF16) for i in range(2)]
    tm1 = fw.sb("r_tm1", [128, NE], F32)
    tm = fw.sb("r_tm", [128, NE], F32)
    td = fw.sb("r_td", [128, NE], F32)
    okm = fw.sb("r_okm", [128, NE], F32)
    x1Tv = self.scr['x1T'].rearrange("(kt p) t -> p kt t", p=128)

    def R0(i):
        xt, xb, lg = xT_[i % 2], xb_[i % 3], lg_[i % 2]
        self.ld(xt.ap[:], x1Tv[:, :, i * 128:(i + 1) * 128], [self.R['x1T']], [xt])
        self.ld(xb.ap[:], self.scr['x1b'][i * 128:(i + 1) * 128, :], [self.R['x1b']], [xb])
        pb = self.bank_from('rL', [0, 1])
        for kt in range(8):
            self.mm(pb.ap[:, 0:36], xt.ap[:, kt, :], wr.ap[:, kt, :], kt == 0, kt == 7, [xt, wr], [pb])
        self.tt(DVE, lg.ap[:], pb.ap[:, 0:36], brt.ap[:], ALU.add, [pb, brt], [lg])

    def R1(i):
        lg, w, top8 = lg_[i % 2], w_[i % 2], top8_[i % 2]
        A1, A2, A, Ab = A1_[i % 2], A2_[i % 2], A_[i % 2], Ab_[i % 2]
        self.red(DVE, w.ap[:, 0:1], lg.ap[:, 0:4], ALU.max, [lg], [w])
        self.ts(DVE, w.ap[:, 1:2], w.ap[:, 0:1], -1.0, None, ALU.mult, None, [w], [w])
        self.ts(DVE, w.ap[:, 4:8], lg.ap[:, 0:4], w.ap[:, 0:1], None, ALU.is_equal, None, [lg, w], [w])
        self.act(w.ap[:, 8:12], lg.ap[:, 0:4], AF.Exp, [lg, w], [w], bias=w.ap[:, 1:2], accum=w.ap[:, 2:3])
        fw.op(DVE, lambda: nc.vector.reciprocal(out=w.ap[:, 3:4], in_=w.ap[:, 2:3]), [w], [w])
        self.ts(DVE, w.ap[:, 12:16], w.ap[:, 4:8], -1.0, 1.0e9, ALU.add, ALU.mult, [w], [w])
        self.tt(DVE, tm1.ap[:].rearrange("p (g e) -> p g e", g=4), lg.ap[:, 4:36].rearrange("p (g e) -> p g e", g=4),
                w.ap[:, 12:16].unsqueeze(2).to_broadcast([128, 4, 8]), ALU.add, [lg, w], [tm1])
        fw.op(DVE, lambda: nc.vector.max(out=top8.ap[:], in_=tm1.ap[:]), [tm1], [top8])
        self.ts(DVE, A1.ap[:], tm1.ap[:], top8.ap[:, 0:1], None, ALU.is_equal, None, [tm1, top8], [A1])
        self.ts(DVE, A2.ap[:], tm1.ap[:], top8.ap[:, 1:2], None, ALU.is_equal, None, [tm1, top8], [A2])
        self.tt(DVE, A.ap[:], A1.ap[:], A2.ap[:], ALU.add, [A1, A2], [A])
        self.cp(DVE, Ab.ap[:], A.ap[:], [A], [Ab])
        self.ts(DVE, w.ap[:, 16:17], top8.ap[:, 0:1], -1.0, None, ALU.mult, None, [top8], [w])
        self.act(w.ap[:, 17:18], top8.ap[:, 1:2], AF.Exp, [top8, w], [w], bias=w.ap[:, 16:17])
        self.ts(DVE, w.ap[:, 18:19], w.ap[:, 17:18], 1.0, None, ALU.add, None, [w], [w])
        fw.op(DVE, lambda: nc.vector.reciprocal(out=w.ap[:, 19:20], in_=w.ap[:, 18:19]), [w], [w])
        self.tt(DVE, WT.ap[:, i, 0:1], w.ap[:, 19:20], w.ap[:, 3:4], ALU.mult, [w], [WT])
        self.tt(DVE, WT.ap[:, i, 1:2], WT.ap[:, i, 0:1], w.ap[:, 17:18], ALU.mult, [w, WT], [WT])

    def R2(i):
        xb, w2 = xb_[i % 3], w2_[i % 2]
        A1, A2, A, Ab = A1_[i % 2], A2_[i % 2], A_[i % 2], Ab_[i % 2]
        pp = self.bank_from('rP', [2, 3])
        self.mm(pp.ap[:, 0:NE], ustr.ap[:], Ab.ap[:], True, False, [ustr, Ab], [pp])
        self.mm(pp.ap[:, 0:NE], self.ones_bf.ap[:], Asb.ap[:], False, True, [self.ones_bf, Asb], [pp])
        self.stt(DVE, td.ap[:], pp.ap[:, 0:NE], float(CAP - 1), ecap.ap[:], ALU.min, ALU.add, [pp, ecap], [td])
        self.ts(DVE, okm.ap[:], pp.ap[:, 0:NE], float(CAP), None, ALU.is_lt, None, [pp], [okm])
        self.tt(DVE, tm.ap[:], A1.ap[:], okm.ap[:], ALU.mult, [A1, okm], [tm])
        self.red(DVE, w2.ap[:, 2:3], tm.ap[:], ALU.add, [tm], [w2])
        self.tt(DVE, tm.ap[:], A2.ap[:], okm.ap[:], ALU.mult, [A2, okm], [tm])
        self.red(DVE, w2.ap[:, 3:4], tm.ap[:], ALU.add, [tm], [w2])
        self.tt(DVE, WT.ap[:, i, :], WT.ap[:, i, :], w2.ap[:, 2:4], ALU.mult, [WT, w2], [WT])
        self.tt(DVE, tm.ap[:], A1.ap[:], td.ap[:], ALU.mult, [A1, td], [tm])
        self.red(DVE, w2.ap[:, 0:1], tm.ap[:], ALU.add, [tm], [w2])
        self.tt(DVE, tm.ap[:], A2.ap[:], td.ap[:], ALU.mult, [A2, td], [tm])
        self.red(DVE, w2.ap[:, 1:2], tm.ap[:], ALU.add, [tm], [w2])
        self.cp(DVE, DSTi.ap[:, i, :], w2.ap[:, 0:2], [w2], [DSTi])
        self.tt(DVE, Asum.ap[:], Asum.ap[:], A.ap[:], ALU.add, [Asum, A], [Asum])
        self.cp(DVE, Asb.ap[:], Asum.ap[:], [Asum], [Asb])
        for j in range(2):
            fw.dma(POOL, None, None, reads=[xb, DSTi], writes=[self.R['xdisp']],
                   fn=lambda j=j: nc.gpsimd.indirect_dma_start(
                       out=self.scr['xdisp'], out_offset=bass.IndirectOffsetOnAxis(ap=DSTi.ap[:, i, j:j + 1], axis=0),
                       in_=xb.ap[:], in_offset=None))

    self.pipeline_fine(NT, [R0, R1, R2])
    self.pop()


Prog.phase_route = _phase_route


def _phase_moe(self, l, parts=("route", "experts", "combine"), last=False):
    self.push()
    fw = self.fw
    self.DSTi = fw.sb("moe_DSTi", [128, NT, 2], I32)
    self.WT = fw.sb("moe_WT", [128, NT, 2], F32)
    if "route" in parts:
        self.phase_route(l)
        if "rinfo" in self.debug:
            dbg = fw.sb("moe_dbg", [128, NT, 4], F32)
            self.cp(self.DVE, dbg.ap[:, :, 0:2], self.DSTi.ap[:], [self.DSTi], [dbg])
            self.cp(self.DVE, dbg.ap[:, :, 2:4], self.WT.ap[:], [self.WT], [dbg])
            self.ld(self.scr['rinfo'], dbg.ap[:].rearrange("p a b -> p (a b)"), [dbg], [self.R['rinfo']])
    if "experts" in parts:
        self.phase_experts(l)
    if "combine" in parts:
        self.phase_combine(l, last)
    self.pop()


Prog.phase_moe = _phase_moe


def _phase_experts(self, l):
    self.push()
    fw, nc = self.fw, self.nc
    DVE, ACT, POOL, PE = self.DVE, self.ACT, self.POOL, self.PE
    inp = self.inp
    NB = 4
    wg = [fw.sb("e_wg%d" % i, [128, 8, 512], BF16) for i in range(NB)]
    wu = [fw.sb("e_wu%d" % i, [128, 8, 512], BF16) for i in range(NB)]
    wd = [fw.sb("e_wd%d" % i, [128, 4, 1024], BF16) for i in range(NB)]
    xr = [fw.sb("e_xr%d" % i, [128, NTB, D], BF16) for i in range(2)]
    xg = [fw.sb("e_xg%d" % i, [128, 8, CAP], BF16) for i in range(2)]
    hT = [fw.sb("e_hT%d" % i, [128, 4, CAP], BF16) for i in range(2)]
    sg = [fw.sb("e_sg%d" % i, [128, CAP], F32) for i in range(2)]
    yo = [fw.sb("e_yo%d" % i, [128, D], F32) for i in range(2)]
    cnt = {'y': 0}

    def E0(e):
        self.ld(xr[e % 2].ap[:], self.scr['xdisp'][e * CS:e * CS + CAP, :].rearrange("(b p) d -> p b d", p=128),
                [self.R['xdisp']], [xr[e % 2]])
        self.ld(wg[e % NB].ap[:], inp['moe_w_gate'][l, e].rearrange("(kt p) c -> p kt c", p=128), [self.R_w], [wg[e % NB]], q=POOL)
        self.ld(wu[e % NB].ap[:], inp['moe_w_up'][l, e].rearrange("(kt p) c -> p kt c", p=128), [self.R_w], [wu[e % NB]], q=POOL)
        self.ld(wd[e % NB].ap[:], inp['moe_w_down'][l, e].rearrange("(kt p) c -> p kt c", p=128), [self.R_w], [wd[e % NB]], q=POOL)

    def E1(e):
        xr_, xg_ = xr[e % 2], xg[e % 2]
        for kt in range(8):
            bT = self.bank_from('eT', [0, 1])
            bTv = bT.ap[:].bitcast(BF16)
            for tb in range(NTB):
                self.tr(bTv[:, tb * 128:(tb + 1) * 128], xr_.ap[:, tb, kt * 128:(kt + 1) * 128], self.ident_bf.ap[:],
                        [xr_, self.ident_bf], [bT])
            self.cp(ACT if kt % 2 else DVE, xg_.ap[:, kt, :], bTv[:, 0:CAP], [bT], [xg_])

    def E2(e):
        xg_, h_ = xg[e % 2], hT[e % 2]
        wg_, wu_ = wg[e % NB], wu[e % NB]
        for ht in range(4):
            bg = self.bank_from('eG', [2, 3, 4, 5])
            for kt in range(8):
                self.mm(bg.ap[:, 0:CAP], wg_.ap[:, kt, ht * 128:(ht + 1) * 128], xg_.ap[:, kt, :], kt == 0, kt == 7, [wg_, xg_], [bg])
            bu = self.bank_from('eG', [2, 3, 4, 5])
            for kt in range(8):
                self.mm(bu.ap[:, 0:CAP], wu_.ap[:, kt, ht * 128:(ht + 1) * 128], xg_.ap[:, kt, :], kt == 0, kt == 7, [wu_, xg_], [bu])
            s_ = sg[ht % 2]
            self.act(s_.ap[:], bg.ap[:, 0:CAP], AF.Silu, [bg], [s_])
            self.tt(DVE, h_.ap[:, ht, :], s_.ap[:], bu.ap[:, 0:CAP], ALU.mult, [s_, bu], [h_])

    def E3(e):
        h_, wd_ = hT[e % 2], wd[e % NB]
        for tb in range(NTB):
            y_ = yo[cnt['y'] % 2]
            cnt['y'] += 1
            for hf in range(2):
                bd = self.bank_from('eD', [6, 7])
                for kt in range(4):
                    self.mm(bd.ap[:], h_.ap[:, kt, tb * 128:(tb + 1) * 128], wd_.ap[:, kt, hf * 512:(hf + 1) * 512],
                            kt == 0, kt == 3, [h_, wd_], [bd])
                self.cp(ACT, y_.ap[:, hf * 512:(hf + 1) * 512], bd.ap[:], [bd], [y_])
            r0 = e * CS + tb * 128
            self.ld(self.scr['ydisp'][r0:r0 + 128, :], y_.ap[:], [y_], [self.R['ydisp']])

    self.pipeline(NE, [E0, E1, E2, E3])
    self.pop()


Prog.phase_experts = _phase_experts


def _phase_combine(self, l, last):
    self.push()
    fw, nc = self.fw, self.nc
    DVE, ACT, POOL, PE = self.DVE, self.ACT, self.POOL, self.PE
    inp = self.inp
    DSTi, WT = self.DSTi, self.WT
    g_bc = fw.sb("c_gbc", [128, D], F32)
    b_bc = fw.sb("c_bbc", [128, D], F32)
    self.ld(g_bc.ap[:], inp['ln2_g'][l].partition_broadcast(128), [self.R_w], [g_bc])
    self.ld(b_bc.ap[:], inp['ln2_b'][l].partition_broadcast(128), [self.R_w], [b_bc])
    xt = [fw.sb("c_xt%d" % i, [128, D], F32) for i in range(3)]
    y1 = [fw.sb("c_y1%d" % i, [128, D], F32) for i in range(3)]
    y2 = [fw.sb("c_y2%d" % i, [128, D], F32) for i in range(3)]
    rt = [fw.sb("c_rt%d" % i, [128, D], F32) for i in range(4)]
    xo = [fw.sb("c_xo%d" % i, [128, D], F32) for i in range(2)]
    xb = [fw.sb("c_xb%d" % i, [128, D], BF16) for i in range(2)]
    stg = [fw.sb("c_stg%d" % i, [128, D], BF16) for i in range(2)]
    junk = fw.sb("c_junk", [128, D], F32)
    st = fw.sb("c_st", [128, 32], F32)

    def C0(i):
        x_, a_, b_ = xt[i % 3], y1[i % 3], y2[i % 3]
        self.ld(x_.ap[:], self.scr['xres1'][i * 128:(i + 1) * 128, :], [self.R['xres1']], [x_])
        for j, y_ in ((0, a_), (1, b_)):
            fw.dma(POOL, None, None, reads=[self.R['ydisp'], DSTi], writes=[y_],
                   fn=lambda j=j, y_=y_: nc.gpsimd.indirect_dma_start(
                       out=y_.ap[:], out_offset=None, in_=self.scr['ydisp'],
                       in_offset=bass.IndirectOffsetOnAxis(ap=DSTi.ap[:, i, j:j + 1], axis=0)))

    def C1(i):
        x_, a_, b_, r_ = xt[i % 3], y1[i % 3], y2[i % 3], rt[i % 4]
        self.ts(DVE, r_.ap[:], a_.ap[:], WT.ap[:, i, 0:1], None, ALU.mult, None, [a_, WT], [r_])
        self.stt(DVE, r_.ap[:], b_.ap[:], WT.ap[:, i, 1:2], r_.ap[:], ALU.mult, ALU.add, [b_, WT, r_], [r_])
        self.stt(DVE, r_.ap[:], x_.ap[:], ALPHA, r_.ap[:], ALU.mult, ALU.add, [x_, r_], [r_])

    def C2(i):
        self.ln_stats(rt[i % 4], st, (i % 4) * 8, junk)

    def C3(i):
        self.ln_rstd(st, (i % 4) * 8)

    def C4(i):
        self.ln_apply(rt[i % 4], st, (i % 4) * 8, g_bc, b_bc, xo[i % 2], None if last else xb[i % 2])

    def C5(i):
        if last:
            self.ld(self.out[i * 128:(i + 1) * 128, :], xo[i % 2].ap[:], [xo[i % 2]], [self.R_out])
        else:
            self.ld(self.scr['xres0'][i * 128:(i + 1) * 128, :], xo[i % 2].ap[:], [xo[i % 2]], [self.R['xres0']])
            self.transpose_store(xb[i % 2].ap, xb[i % 2], self.scr['xT'], self.R['xT'], i, stg, i)

    self.pipeline_fine(NT, [C0, C1, C2, C3, C4, C5])
    self.pop()


Prog.phase_combine = _phase_combine


N_CORES = 8
FUSED = True


def build_program(L, first=True):
    P = Prog(L)
    if first:
        P.phase_t0()
    for l in range(L):
        P.phase_inproj(l)
        P.phase_mlstm(l)
        P.phase_fox(l)
        P.phase_s5(l)
        P.phase_merge(l)
        P.phase_moe(l, last=(l == L - 1))
    return P.finish()


def kernel(**inputs):
    x = np.ascontiguousarray(np.asarray(inputs['x'], dtype=np.float32))
    consts = host_consts()
    if FUSED:
        nc = build_program(DEPTH)
        in_maps = []
        for b in range(N_CORES):
            m = {'x': x[b]}
            for n in INPUT_NAMES:
                m[n] = np.ascontiguousarray(np.asarray(inputs[n], dtype=np.float32))
            m.update(consts)
            in_maps.append(m)
        res = run_bass_kernel_spmd(nc, in_maps, core_ids=list(range(N_CORES)))
        return np.stack([np.asarray(res.results[b]['out'], dtype=np.float32) for b in range(N_CORES)])
    nc = build_program(1)
    cur = x
    for l in range(DEPTH):
        w = {n: np.ascontiguousarray(np.asarray(inputs[n], dtype=np.float32)[l:l + 1]) for n in INPUT_NAMES}
        in_maps = []
        for b in range(N_CORES):
            m = {'x': np.ascontiguousarray(cur[b])}
            m.update(w)
            m.update(consts)
            in_maps.append(m)
        res = run_bass_kernel_spmd(nc, in_maps, core_ids=list(range(N_CORES)))
        cur = np.stack([np.asarray(res.results[b]['out'], dtype=np.float32) for b in range(N_CORES)])
    return cur
```

```python
import numpy as np
import concourse.bass as bass
import concourse.mybir as mybir
from concourse.bass_utils import run_bass_kernel_spmd

F32 = mybir.dt.float32
BF16 = mybir.dt.bfloat16
I32 = mybir.dt.int32
U32 = mybir.dt.uint32
AF = mybir.ActivationFunctionType
ALU = mybir.AluOpType
AX = mybir.AxisListType


class Res:
    __slots__ = ("name", "last_w", "readers", "ap")

    def __init__(self, name, ap=None):
        self.name = name
        self.last_w = None
        self.readers = {}
        self.ap = ap


class Eng:
    def __init__(self, name, handle, sem, semkey):
        self.name = name
        self.h = handle
        self.sem = sem
        self.semkey = semkey
        self.count = 0
        self.seen = {}
        self.ring = []
        self.dma_count = 0


class FW:
    RING = 8

    def __init__(self, nc):
        self.nc = nc
        self.sems = {}
        self._ctx = []
        self.n_instr = 0

        def mk(name):
            cm = nc.semaphore(name)
            s = cm.__enter__()
            self._ctx.append(cm)
            self.sems[name] = s
            return s

        self.PE = Eng("pe", nc.tensor, mk("s_pe"), "s_pe")
        self.DVE = Eng("dve", nc.vector, mk("s_dve"), "s_dve")
        self.ACT = Eng("act", nc.scalar, mk("s_act"), "s_act")
        self.POOL = Eng("pool", nc.gpsimd, mk("s_pool"), "s_pool")
        self.SP = Eng("sp", nc.sync, mk("s_sp"), "s_sp")
        for e in (self.SP, self.ACT, self.POOL):
            for i in range(4 if e is self.POOL else self.RING):
                k = "r_%s_%d" % (e.name, i)
                mk(k)
                e.ring.append(k)
        self.engines = [self.PE, self.DVE, self.ACT, self.POOL, self.SP]

    def res(self, name, ap=None):
        return Res(name, ap)

    def sb(self, name, shape, dtype):
        self._uid = getattr(self, '_uid', 0) + 1
        name = "%s_u%d" % (name, self._uid)
        cm = self.nc.sbuf_tensor(name, shape, dtype)
        t = cm.__enter__()
        self._ctx.append(cm)
        return Res(name, t)

    def ps(self, name, shape, dtype):
        cm = self.nc.psum_tensor(name, shape, dtype)
        t = cm.__enter__()
        self._ctx.append(cm)
        return Res(name, t)

    def _wait(self, eng, deps):
        best = {}
        for d in deps:
            if d is None:
                continue
            k, v = d
            if best.get(k, 0) < v:
                best[k] = v
        for k, v in best.items():
            if eng is self.PE and k == "s_pe":
                continue
            if eng.seen.get(k, 0) < v:
                eng.h.wait_ge(self.sems[k], v)
                eng.seen[k] = v

    def _deps(self, reads, writes):
        deps = []
        for r in reads:
            deps.append(r.last_w)
        for w in writes:
            deps.append(w.last_w)
            deps.extend(w.readers.items())
        return deps

    def _commit(self, ev, reads, writes):
        k, v = ev
        for r in reads:
            if r.readers.get(k, 0) < v:
                r.readers[k] = v
        for w in writes:
            w.last_w = ev
            w.readers = {}

    def op(self, eng, fn, reads=(), writes=()):
        if getattr(self, '_defer', None) is not None:
            self._defer.append((self._op_now, (eng, fn, tuple(reads), tuple(writes)), {}))
            return None
        return self._op_now(eng, fn, reads, writes)

    def _op_now(self, eng, fn, reads=(), writes=()):
        self._wait(eng, self._deps(reads, writes))
        ins = fn()
        eng.count += 1
        ins.then_inc(eng.sem, 1)
        self.n_instr += 1
        self._commit((eng.semkey, eng.count), reads, writes)
        return ins

    def dma(self, q, out, in_, reads=(), writes=(), fn=None, **kw):
        if getattr(self, '_defer', None) is not None:
            kw2 = dict(kw)
            kw2.update(reads=tuple(reads), writes=tuple(writes), fn=fn)
            self._defer.append((self._dma_now, (q, out, in_), kw2))
            return None
        return self._dma_now(q, out, in_, reads=reads, writes=writes, fn=fn, **kw)

    def _dma_now(self, q, out, in_, reads=(), writes=(), fn=None, **kw):
        deps = self._deps(reads, writes)
        k = q.dma_count
        nr = len(q.ring)
        slot = q.ring[k % nr]
        rnd = k // nr
        if rnd > 0:
            deps.append((slot, 16 * rnd))
        self._wait(q, deps)
        if fn is None:
            ins = q.h.dma_start(out=out, in_=in_, **kw)
        else:
            ins = fn()
        ins.then_inc(self.sems[slot], 16)
        q.dma_count += 1
        self.n_instr += 1
        self._commit((slot, 16 * (rnd + 1)), reads, writes)
        return ins

    def finish(self, outs):
        deps = []
        for o in outs:
            deps.append(o.last_w)
        for e in (self.SP, self.ACT, self.POOL):
            k = e.dma_count
            nr = len(e.ring)
            for i in range(min(k, nr)):
                j = k - 1 - i
                deps.append((e.ring[j % nr], 16 * (j // nr + 1)))
        for e in (self.PE, self.DVE, self.ACT, self.POOL):
            if e.count:
                deps.append((e.semkey, e.count))
        self._wait(self.SP, deps)


S = 4096
D = 1024
W = 512
NT = 32
NCH = 8
IN_TOTAL = 7184
ST = [0, 1024, 1536, 2048, 2052, 2056, 2568, 4104, 4112, 7184]
DEPTH = 4
ALPHA = (2 * DEPTH) ** 0.25
LN_EPS = 1e-5
NE = 32
CAP = 384
NTB = CAP // 128
CS = CAP + 128
TWO_PI = 6.283185307179586

INPUT_NAMES = ['w_in', 'b_in', 'm_conv_w', 'm_conv_b', 'm_norm_w', 's5_lam_re', 's5_lam_im',
               's5_log_dt', 's5_b_re', 's5_b_im', 's5_c_re', 's5_c_im', 's5_d', 's5_w_glu',
               's5_b_glu', 'w_branch', 'w_out', 'ln1_g', 'ln1_b', 'w_route_group',
               'b_route_group', 'w_route_expert', 'b_route_expert', 'moe_w_gate', 'moe_w_up',
               'moe_w_down', 'ln2_g', 'ln2_b']
INPUT_SHAPES = {
    'w_in': (1024, 7184), 'b_in': (7184,), 'm_conv_w': (4, 1024), 'm_conv_b': (1024,),
    'm_norm_w': (512,), 's5_lam_re': (32, 64), 's5_lam_im': (32, 64), 's5_log_dt': (32,),
    's5_b_re': (32, 64, 16), 's5_b_im': (32, 64, 16), 's5_c_re': (32, 16, 64),
    's5_c_im': (32, 16, 64), 's5_d': (32, 16), 's5_w_glu': (512, 512), 's5_b_glu': (512,),
    'w_branch': (3, 512, 1024), 'w_out': (1024, 1024), 'ln1_g': (1024,), 'ln1_b': (1024,),
    'w_route_group': (1024, 4), 'b_route_group': (4,), 'w_route_expert': (1024, 32),
    'b_route_expert': (32,), 'moe_w_gate': (32, 1024, 512), 'moe_w_up': (32, 1024, 512),
    'moe_w_down': (32, 512, 1024), 'ln2_g': (1024,), 'ln2_b': (1024,),
}


def host_consts():
    import ml_dtypes
    c = {}
    c['c_ident_bf'] = np.eye(128, dtype=np.float32).astype(ml_dtypes.bfloat16)
    c['c_ident_f'] = np.eye(128, dtype=np.float32)
    s_idx = np.arange(128)[:, None]
    t_idx = np.arange(128)[None, :]
    m01 = (s_idx <= t_idx).astype(np.float32)
    c['c_mask01x4'] = np.tile(m01, (1, 4))
    c['c_maskneg'] = np.where(t_idx <= s_idx, 0.0, -30000.0).astype(np.float32)
    c['c_ustrict'] = (s_idx < t_idx).astype(np.float32).astype(ml_dtypes.bfloat16)
    c['c_ones_bf'] = np.ones((128, 128), np.float32).astype(ml_dtypes.bfloat16)
    sel = np.zeros((8, 8 * 128), np.float32)
    for h in range(8):
        sel[h, h * 128:(h + 1) * 128] = 1.0
    c['c_sel8'] = sel
    c['c_iota'] = np.tile(np.arange(S, dtype=np.float32)[None, :], (128, 1))
    esel = np.zeros((128, 8, 16), np.float32)
    for k in range(128):
        esel[k, k // 16, k % 16] = 1.0
    c['c_esel'] = esel
    c['c_ecap'] = np.tile((np.arange(NE, dtype=np.float32) * CS)[None, :], (128, 1))
    return c


CONST_SPECS = {
    'c_ident_bf': ([128, 128], BF16), 'c_ident_f': ([128, 128], F32),
    'c_mask01x4': ([128, 512], F32), 'c_maskneg': ([128, 128], F32),
    'c_ustrict': ([128, 128], BF16), 'c_ones_bf': ([128, 128], BF16),
    'c_sel8': ([8, 1024], F32), 'c_iota': ([128, S], F32), 'c_esel': ([128, 8, 16], F32),
    'c_ecap': ([128, NE], F32),
}


class Prog:
    def __init__(self, L, debug=(), stop_after=None):
        self.L = L
        self.debug = set(debug)
        self.stop_after = stop_after
        nc = self.nc = bass.Bass("TRN2", target_bir_lowering=False)
        fw = self.fw = FW(nc)
        self.PE, self.DVE, self.ACT, self.POOL, self.SP = fw.PE, fw.DVE, fw.ACT, fw.POOL, fw.SP
        self.inp = {}
        self.x_in = nc.dram_tensor("x", [S, D], F32, kind="ExternalInput").ap()
        self.R_xin = fw.res("x")
        for n in INPUT_NAMES:
            self.inp[n] = nc.dram_tensor(n, [L] + list(INPUT_SHAPES[n]), F32, kind="ExternalInput").ap()
        self.R_w = fw.res("weights")
        self.cst = {}
        for n, (shp, dt) in CONST_SPECS.items():
            self.cst[n] = nc.dram_tensor(n, shp, dt, kind="ExternalInput").ap()
        self.out = nc.dram_tensor("out", [S, D], F32, kind="ExternalOutput").ap()
        self.R_out = fw.res("out")
        self.scr = {}
        self.R = {}
        for n, shp, dt in [
            ("xT", [D, S], BF16), ("xres1", [S, D], F32), ("xres0", [S, D], F32),
            ("x1T", [D, S], BF16), ("x1b", [S, D], BF16),
            ("qkT", [1024, S], BF16), ("moT", [512, S], F32), ("suT", [512, S], BF16),
            ("aqT", [512, S], BF16), ("akT", [512, S], BF16), ("gT", [3072, S], F32),
            ("gsm", [16, S], F32), ("vm", [S, 512], BF16), ("va", [S, 512], BF16),
            ("ymT", [512, S], BF16), ("ysT", [512, S], BF16), ("yaT", [512, S], BF16),
            ("s5y", [512, S], F32), ("fx", [4, 8, S], BF16),
            ("xdisp", [NE * CS, D], BF16), ("ydisp", [NE * CS, D], F32), ("rinfo", [128, NT * 4], F32),
        ]:
            kind = "ExternalOutput" if n in self.debug else "Internal"
            self.scr[n] = nc.dram_tensor(n, shp, dt, kind=kind).ap()
            self.R[n] = fw.res(n)
        self.ident_bf = fw.sb("ident_bf", [128, 128], BF16)
        self.ident_f = fw.sb("ident_f", [128, 128], F32)
        self.ones_bf = fw.sb("ones_bf", [128, 128], BF16)
        self.R_c = fw.res("consts")
        self.eps_col = fw.sb("eps_col", [128, 1], F32)
        self.memset(self.DVE, self.eps_col.ap[:], LN_EPS, [self.eps_col])
        self.ld(self.ident_bf.ap[:], self.cst['c_ident_bf'], [self.R_c], [self.ident_bf])
        self.ld(self.ident_f.ap[:], self.cst['c_ident_f'], [self.R_c], [self.ident_f])
        self.ld(self.ones_bf.ap[:], self.cst['c_ones_bf'], [self.R_c], [self.ones_bf])
        self.banks = [fw.ps("bank%d" % i, [128, 512], F32) for i in range(8)]
        self._bank_i = 0
        self._marks = []

    def bank(self):
        b = self.banks[self._bank_i % 8]
        self._bank_i += 1
        return b

    def push(self):
        self._marks.append(len(self.fw._ctx))

    def pop(self):
        self.barrier()
        m = self._marks.pop()
        while len(self.fw._ctx) > m:
            cm = self.fw._ctx.pop()
            cm.__exit__(None, None, None)

    def barrier(self):
        fw = self.fw
        deps = []
        for e in (fw.SP, fw.ACT, fw.POOL):
            k = e.dma_count
            nr = len(e.ring)
            for i in range(min(k, nr)):
                j = k - 1 - i
                deps.append((e.ring[j % nr], 16 * (j // nr + 1)))
        for e in (fw.PE, fw.DVE, fw.ACT, fw.POOL):
            if e.count:
                deps.append((e.semkey, e.count))
        for e in fw.engines:
            fw._wait(e, deps)

    def ld(self, out, in_, reads, writes, q=None, **kw):
        return self.fw.dma(q or self.SP, out, in_, reads=reads, writes=writes, **kw)

    def mm(self, out, lhsT, rhs, start, stop, reads, writes):
        nc = self.nc
        return self.fw.op(self.PE, lambda: nc.tensor.matmul(out, lhsT, rhs, start=start, stop=stop),
                          reads, writes)

    def tr(self, out, in_, ident, reads, writes):
        nc = self.nc
        return self.fw.op(self.PE, lambda: nc.tensor.transpose(out, in_, ident), reads, writes)

    def act(self, out, in_, func, reads, writes, bias=None, scale=1.0, accum=None):
        nc = self.nc
        kw = {}
        if bias is not None:
            kw['bias'] = bias
        if accum is not None:
            kw['accum_out'] = accum
        return self.fw.op(self.ACT, lambda: nc.scalar.activation(out=out, in_=in_, func=func, scale=scale, **kw),
                          reads, writes)

    def tt(self, eng, out, in0, in1, op, reads, writes):
        return self.fw.op(eng, lambda: eng.h.tensor_tensor(out=out, in0=in0, in1=in1, op=op), reads, writes)

    def ts(self, eng, out, in0, s1, s2, op0, op1, reads, writes, accum=None):
        kw = {}
        if accum is not None:
            kw['accum_out'] = accum
        if s2 is None:
            return self.fw.op(eng, lambda: eng.h.tensor_scalar(out=out, in0=in0, scalar1=s1, scalar2=None, op0=op0, **kw),
                              reads, writes)
        return self.fw.op(eng, lambda: eng.h.tensor_scalar(out=out, in0=in0, scalar1=s1, scalar2=s2, op0=op0, op1=op1, **kw),
                          reads, writes)

    def stt(self, eng, out, in0, scalar, in1, op0, op1, reads, writes):
        return self.fw.op(eng, lambda: eng.h.scalar_tensor_tensor(out=out, in0=in0, scalar=scalar, in1=in1, op0=op0, op1=op1),
                          reads, writes)

    def cp(self, eng, out, in_, reads, writes):
        if eng is self.ACT:
            return self.act(out, in_, AF.Copy, reads, writes)
        return self.fw.op(eng, lambda: eng.h.tensor_copy(out=out, in_=in_), reads, writes)

    def memset(self, eng, ap, val, writes):
        return self.fw.op(eng, lambda: eng.h.memset(ap, val), (), writes)

    def red(self, eng, out, in_, op, reads, writes, axis=None):
        axis = axis or AX.X
        return self.fw.op(eng, lambda: eng.h.tensor_reduce(out=out, in_=in_, axis=axis, op=op), reads, writes)

    def col_load(self, tile_res, col, vec_ap, n):
        self.ld(tile_res.ap[0:n, col:col + 1], vec_ap.rearrange("(p o) -> p o", o=1), [self.R_w], [tile_res])

    def transpose_store(self, src_bf, src_res, dstT, dstT_res, i, stage, stage_i, pool=None, q=None):
        b = self.bank_from(*pool) if pool else self.bank()
        pv = b.ap[:].bitcast(BF16)
        for kt in range(8):
            self.tr(pv[:, kt * 128:(kt + 1) * 128], src_bf[:, kt * 128:(kt + 1) * 128], self.ident_bf.ap[:],
                    [src_res, self.ident_bf], [b])
        st = stage[stage_i % len(stage)]
        self.cp(self.ACT, st.ap[:], pv, [b], [st])
        self.ld(dstT.rearrange("(kt p) t -> p kt t", p=128)[:, :, i * 128:(i + 1) * 128],
                st.ap[:].rearrange("p (kt t) -> p kt t", kt=8), [st], [dstT_res], q=q)

    def phase_t0(self):
        self.push()
        fw = self.fw
        xt = [fw.sb("t0x%d" % i, [128, D], F32) for i in range(3)]
        xb = [fw.sb("t0b%d" % i, [128, D], BF16) for i in range(2)]
        stg = [fw.sb("t0s%d" % i, [128, D], BF16) for i in range(2)]

        def A(i):
            t = xt[i % 3]
            self.ld(t.ap[:], self.x_in[i * 128:(i + 1) * 128, :], [self.R_xin], [t])

        def B(i):
            t = xt[i % 3]
            self.ld(self.scr['xres0'][i * 128:(i + 1) * 128, :], t.ap[:], [t], [self.R['xres0']])
            self.cp(self.DVE, xb[i % 2].ap[:], t.ap[:], [t], [xb[i % 2]])

        def C(i):
            self.transpose_store(xb[i % 2].ap, xb[i % 2], self.scr['xT'], self.R['xT'], i, stg, i)

        self.pipeline(NT, [A, B, C])
        self.pop()

    def phase_inproj(self, l):
        self.push()
        fw, nc = self.fw, self.nc
        w_in = self.inp['w_in'][l]
        b_in = self.inp['b_in'][l]
        xT = fw.sb("p1_xT", [128, 8, S], BF16)
        self.ld(xT.ap[:], self.scr['xT'].rearrange("(kt p) t -> p kt t", p=128), [self.R['xT']], [xT])
        wbf = [fw.sb("p1_wbf%d" % i, [128, 8, 128], BF16) for i in range(3)]
        rowf = fw.sb("p1_rowf", [128, S + 4], F32)
        acc = fw.sb("p1_acc", [128, S], F32)
        rowo = [fw.sb("p1_rowo%d" % i, [128, S], F32) for i in range(2)]
        bcol = fw.sb("p1_bcol", [128, 64], F32)
        cw = fw.sb("p1_cw", [128, 8, 4], F32)
        cb = fw.sb("p1_cb", [128, 8], F32)
        self.memset(self.DVE, rowf.ap[:, 0:4], 0.0, [rowf])
        for j in range(4):
            self.ld(cw.ap[:, :, j], self.inp['m_conv_w'][l][j].rearrange("(t p) -> p t", p=128), [self.R_w], [cw],
                    allow_slow_non_contiguous=True)
        self.ld(cb.ap[:], self.inp['m_conv_b'][l].rearrange("(t p) -> p t", p=128), [self.R_w], [cb],
                allow_slow_non_contiguous=True)
        tiles = []
        for t in range(8):
            tiles.append((ST[0] + t * 128, 128, "conv", "qkT", t * 128, t))
        for t in range(4):
            tiles.append((ST[2] + t * 128, 128, "sig32", "moT", t * 128, 0))
        for t in range(4):
            tiles.append((ST[5] + t * 128, 128, "bf", "suT", t * 128, 0))
        for t in range(4):
            tiles.append((ST[6] + t * 128, 128, "bfq", "aqT", t * 128, 0))
        for t in range(4):
            tiles.append((ST[6] + 512 + t * 128, 128, "bf", "akT", t * 128, 0))
        for t in range(24):
            tiles.append((ST[8] + t * 128, 128, "sig32", "gT", t * 128, 0))
        tiles.append((None, 16, "small", "gsm", 0, 0))
        for ti, (c0, n, kind, dn, r0, aux) in enumerate(tiles):
            if kind == "small":
                self.col_load(bcol, ti, b_in[ST[3]:ST[3] + 8], 8)
                self.ld(bcol.ap[8:16, ti:ti + 1], b_in[ST[7]:ST[7] + 8].rearrange("(p o) -> p o", o=1),
                        [self.R_w], [bcol])
            else:
                self.col_load(bcol, ti, b_in[c0:c0 + n], n)
        bq = fw.sb("p1_bq", [128, 4], F32)

        def load_w(ti):
            c0, n, kind, dn, r0, aux = tiles[ti]
            ws = wbf[ti % 3]
            if kind == "small":
                self.ld(ws.ap[:, :, 0:8], w_in[:, ST[3]:ST[3] + 8].rearrange("(kt p) c -> p kt c", p=128),
                        [self.R_w], [ws], q=self.POOL)
                self.ld(ws.ap[:, :, 8:16], w_in[:, ST[7]:ST[7] + 8].rearrange("(kt p) c -> p kt c", p=128),
                        [self.R_w], [ws], q=self.POOL)
            else:
                self.ld(ws.ap[:, :, 0:n], w_in[:, c0:c0 + n].rearrange("(kt p) c -> p kt c", p=128),
                        [self.R_w], [ws], q=self.POOL)

        load_w(0)
        load_w(1)
        for ti, (c0, n, kind, dn, r0, aux) in enumerate(tiles):
            if ti + 2 < len(tiles):
                load_w(ti + 2)
            wb = wbf[ti % 3]
            ro = rowo[ti % 2]
            ro_bf = ro.ap[:].bitcast(BF16)
            bc = bcol.ap[0:n, ti:ti + 1]
            for tc in range(NCH):
                b = self.bank()
                for kt in range(8):
                    self.mm(b.ap[0:n, :], wb.ap[:, kt, 0:n], xT.ap[:, kt, tc * 512:(tc + 1) * 512],
                            kt == 0, kt == 7, [wb, xT], [b])
                sl = slice(tc * 512, (tc + 1) * 512)
                if kind == "conv":
                    self.act(rowf.ap[0:n, 3 + tc * 512: 3 + (tc + 1) * 512], b.ap[0:n, :], AF.Identity,
                             [b, bcol], [rowf], bias=bc)
                elif kind == "sig32":
                    self.act(ro.ap[0:n, sl], b.ap[0:n, :], AF.Sigmoid, [b, bcol], [ro], bias=bc)
                elif kind == "bf":
                    self.act(ro_bf[0:n, sl], b.ap[0:n, :], AF.Identity, [b, bcol], [ro], bias=bc)
                elif kind == "bfq":
                    if tc == 0:
                        self.ts(self.DVE, bq.ap[:, 0:1], bc, 0.125, None, ALU.mult, None, [bcol], [bq])
                    self.act(ro_bf[0:n, sl], b.ap[0:n, :], AF.Identity, [b, bq], [ro], bias=bq.ap[:, 0:1], scale=0.125)
                elif kind == "small":
                    self.act(ro.ap[0:n, sl], b.ap[0:n, :], AF.Identity, [b, bcol], [ro], bias=bc)
            dst = self.scr[dn]
            if kind == "conv":
                t = aux
                self.ts(self.DVE, acc.ap[:], rowf.ap[:, 0:S], cw.ap[:, t, 0:1], cb.ap[:, t:t + 1], ALU.mult, ALU.add,
                        [rowf, cw, cb], [acc])
                for j in range(1, 4):
                    self.stt(self.DVE, acc.ap[:], rowf.ap[:, j:j + S], cw.ap[:, t, j:j + 1], acc.ap[:], ALU.mult, ALU.add,
                             [rowf, cw, acc], [acc])
                self.act(ro_bf[:, 0:S], acc.ap[:], AF.Silu, [acc], [ro])
                self.ld(dst[r0:r0 + 128, :], ro_bf[:, 0:S], [ro], [self.R[dn]])
            elif kind in ("bf", "bfq"):
                self.ld(dst[r0:r0 + n, :], ro_bf[0:n, 0:S], [ro], [self.R[dn]])
            else:
                self.ld(dst[r0:r0 + n, :], ro.ap[0:n, :], [ro], [self.R[dn]])
        wvb = [fw.sb("p1_wvb%d" % i, [128, 8, 512], BF16) for i in range(2)]
        bvb = [fw.sb("p1_bvb%d" % i, [128, 512], F32) for i in range(2)]
        vout = [fw.sb("p1_vout%d" % i, [128, 512], BF16) for i in range(2)]
        for vi, (c0, dn) in enumerate([(ST[1], "vm"), (ST[6] + 1024, "va")]):
            self.ld(wvb[vi].ap[:], w_in[:, c0:c0 + 512].rearrange("(kt p) c -> p kt c", p=128), [self.R_w], [wvb[vi]],
                    q=self.POOL)
            self.ld(bvb[vi].ap[:], b_in[c0:c0 + 512].partition_broadcast(128), [self.R_w], [bvb[vi]])
            for i in range(NT):
                b = self.bank()
                for kt in range(8):
                    self.mm(b.ap[:], xT.ap[:, kt, i * 128:(i + 1) * 128], wvb[vi].ap[:, kt, :], kt == 0, kt == 7,
                            [xT, wvb[vi]], [b])
                vo = vout[i % 2]
                self.tt(self.DVE, vo.ap[:], b.ap[:], bvb[vi].ap[:], ALU.add, [b, bvb[vi]], [vo])
                self.ld(self.scr[dn][i * 128:(i + 1) * 128, :], vo.ap[:], [vo], [self.R[dn]])
        self.pop()

    def finish(self):
        self.fw.finish([self.R_out])
        return self.nc


def _phase_mlstm(self, l):
    self.push()
    fw, nc = self.fw, self.nc
    DVE, ACT, POOL, PE = self.DVE, self.ACT, self.POOL, self.PE
    sel = fw.sb("m_sel", [4, 512], F32)
    self.ld(sel.ap[:], self.cst['c_sel8'][0:4, 0:512], [self.R_c], [sel])
    mask4 = fw.sb("m_mask4", [128, 512], F32)
    self.ld(mask4.ap[:], self.cst['c_mask01x4'], [self.R_c], [mask4])
    normw = fw.sb("m_normw", [128, 4], F32)
    self.ld(normw.ap[:], self.inp['m_norm_w'][l].rearrange("(h p) -> p h", p=128), [self.R_w], [normw],
            allow_slow_non_contiguous=True)
    QT = fw.sb("m_QT", [128, 4, S], BF16)
    KT = fw.sb("m_KT", [128, 4, S], BF16)
    self.ld(QT.ap[:], self.scr['qkT'][0:512, :].rearrange("(h p) t -> p h t", p=128), [self.R['qkT']], [QT])
    self.ld(KT.ap[:], self.scr['qkT'][512:1024, :].rearrange("(h p) t -> p h t", p=128), [self.R['qkT']], [KT])
    eBb = fw.sb("m_eBb", [128, 4, NT], F32)
    self.push()
    Gi = fw.sb("m_Gi", [4, S], F32)
    Gf = fw.sb("m_Gf", [4, S], F32)
    self.ld(Gi.ap[:], self.scr['gsm'][0:4, :], [self.R['gsm']], [Gi])
    self.ld(Gf.ap[:], self.scr['gsm'][4:8, :], [self.R['gsm']], [Gf])
    E = Gf
    RM = fw.sb("m_RM", [4, S], F32)
    B = fw.sb("m_B", [4, S], F32)
    EQ = fw.sb("m_EQ", [4, S], F32)
    EK = Gi
    EB = fw.sb("m_EB", [4, NT], F32)
    self.act(E.ap[:], Gf.ap[:], AF.Exp, [Gf], [E], scale=-1.0)
    self.act(E.ap[:], E.ap[:], AF.Ln, [E], [E], bias=1.0)
    self.memset(DVE, RM.ap[:], 1.0, [RM])
    self.memset(DVE, RM.ap[:].rearrange("p (c s) -> p c s", s=128)[:, :, 0:1], 0.0, [RM])
    fw.op(DVE, lambda: nc.vector.tensor_tensor_scan(out=B.ap[:], data0=RM.ap[:], data1=E.ap[:], initial=0.0,
                                                    op0=ALU.mult, op1=ALU.subtract), [RM, E], [B])
    self.act(EQ.ap[:], B.ap[:], AF.Exp, [B], [EQ])
    self.tt(DVE, EK.ap[:], Gi.ap[:], B.ap[:], ALU.subtract, [Gi, B], [EK])
    self.act(EK.ap[:], EK.ap[:], AF.Exp, [EK], [EK], bias=float(np.log(128.0 ** -0.5)))
    self.cp(DVE, EB.ap[:], EQ.ap[:].rearrange("p (c s) -> p c s", s=128)[:, :, 127], [EQ], [EB])
    for h in range(4):
        b = self.bank()
        self.mm(b.ap[:, 0:NT], sel.ap[0:4, h * 128:(h + 1) * 128], EB.ap[0:4, :], True, True, [sel, EB], [b])
        self.cp(DVE, eBb.ap[:, h, :], b.ap[:, 0:NT], [b], [eBb])
        for (src, T) in ((EQ, QT), (EK, KT)):
            for tc in range(NCH):
                b = self.bank()
                sl = slice(tc * 512, (tc + 1) * 512)
                self.mm(b.ap[:], sel.ap[0:4, h * 128:(h + 1) * 128], src.ap[0:4, sl], True, True, [sel, src], [b])
                self.tt(DVE, T.ap[:, h, sl], T.ap[:, h, sl], b.ap[:], ALU.mult, [T, b], [T])
    self.pop()
    Vaug = fw.sb("m_V", [128, NT, 4, 129], BF16)
    for h in range(4):
        self.ld(Vaug.ap[:, :, h, 0:128], self.scr['vm'][:, h * 128:(h + 1) * 128].rearrange("(nt p) c -> p nt c", p=128),
                [self.R['vm']], [Vaug])
    self.memset(POOL, Vaug.ap[:, :, :, 128:129], 1.0, [Vaug])
    P32 = fw.sb("m_P32", [128, 4, 129], F32)
    Cbf = [fw.sb("m_Cbf%d" % i, [128, 4, 129], BF16) for i in range(3)]
    self.memset(DVE, P32.ap[:], 0.0, [P32])
    self.memset(DVE, Cbf[0].ap[:], 0.0, [Cbf[0]])
    Ktok = [fw.sb("m_Ktok%d" % i, [128, 512], BF16) for i in range(2)]
    STb = [fw.sb("m_STb%d" % i, [128, 512], BF16) for i in range(3)]
    h32 = [fw.sb("m_h32%d" % i, [128, 4, 128], F32) for i in range(6)]
    sq = [fw.sb("m_sq%d" % i, [128, 4, 128], F32) for i in range(2)]
    hnb = [fw.sb("m_hnb%d" % i, [128, 4, 128], BF16) for i in range(2)]
    st2 = [fw.sb("m_st2%d" % i, [128, 16], F32) for i in range(2)]
    st3 = [fw.sb("m_st3%d" % i, [128, 16], F32) for i in range(6)]
    mo = [fw.sb("m_mo%d" % i, [128, 4, 128], F32) for i in range(2)]
    ymo = [fw.sb("m_ymo%d" % i, [128, 4, 128], BF16) for i in range(2)]
    moT_v = self.scr['moT'].rearrange("(h p) t -> p h t", p=128)
    ymT_v = self.scr['ymT'].rearrange("(h p) t -> p h t", p=128)
    bDs = {}

    def M0(c):
        csl = slice(c * 128, (c + 1) * 128)
        bT = self.bank_from('mT0', [0])
        bTv = bT.ap[:].bitcast(BF16)
        for h in range(4):
            self.tr(bTv[:, h * 128:(h + 1) * 128], KT.ap[:, h, csl], self.ident_bf.ap[:], [KT, self.ident_bf], [bT])
        kt_ = Ktok[c % 2]
        self.cp(ACT, kt_.ap[:], bTv[:, 0:512], [bT], [kt_])
        bS = self.bank_from('mS', [1])
        for h in range(4):
            self.mm(bS.ap[:, h * 128:(h + 1) * 128], KT.ap[:, h, csl], QT.ap[:, h, csl], True, True, [KT, QT], [bS])
        stb = STb[c % 3]
        self.tt(DVE, stb.ap[:], bS.ap[:], mask4.ap[:], ALU.mult, [bS, mask4], [stb])
        bD = [self.bank_from('mD', [2, 3]), self.bank_from('mD', [2, 3])]
        bDs[c] = bD
        for h in range(4):
            o = bD[h // 2].ap[:, (h % 2) * 129:(h % 2) * 129 + 129]
            self.mm(o, kt_.ap[:, h * 128:(h + 1) * 128], Vaug.ap[:, c, h, :], True, True, [kt_, Vaug], [bD[h // 2]])

    def M1(c):
        bD = bDs.pop(c)
        cn = Cbf[(c + 1) % 3]
        for h in range(4):
            o = bD[h // 2].ap[:, (h % 2) * 129:(h % 2) * 129 + 129]
            cprev = max(c - 1, 0)
            self.stt(DVE, P32.ap[:, h, :], P32.ap[:, h, :], eBb.ap[:, h, cprev:cprev + 1], o, ALU.mult, ALU.add,
                     [P32, eBb, bD[h // 2]], [P32])
            self.act(cn.ap[:, h, :], P32.ap[:, h, :], AF.Identity, [P32, eBb], [cn], scale=eBb.ap[:, h, c:c + 1])

    def M2(c):
        csl = slice(c * 128, (c + 1) * 128)
        stb = STb[c % 3]
        cc = Cbf[c % 3]
        st = st2[c % 2]
        h_ = h32[c % 6]
        bN = [self.bank_from('mN', [4, 5]), self.bank_from('mN', [4, 5])]
        for h in range(4):
            o = bN[h // 2].ap[:, (h % 2) * 129:(h % 2) * 129 + 129]
            self.mm(o, stb.ap[:, h * 128:(h + 1) * 128], Vaug.ap[:, c, h, :], True, False, [stb, Vaug], [bN[h // 2]])
            self.mm(o, QT.ap[:, h, csl], cc.ap[:, h, :], False, True, [QT, cc], [bN[h // 2]])
        for j in range(2):
            den = bN[j].ap[:, 0:258].rearrange("p (a b) -> p a b", b=129)[:, :, 128]
            self.ts(DVE, st.ap[:, 8 + 2 * j:10 + 2 * j], den, -1.0, None, ALU.mult, None, [bN[j]], [st])
            self.tt(DVE, st.ap[:, 2 * j:2 * j + 2], den, st.ap[:, 8 + 2 * j:10 + 2 * j], ALU.max, [bN[j], st], [st])
        self.ts(DVE, st.ap[:, 0:4], st.ap[:, 0:4], 1.0, None, ALU.max, None, [st], [st])
        fw.op(DVE, lambda: nc.vector.reciprocal(out=st.ap[:, 4:8], in_=st.ap[:, 0:4]), [st], [st])
        for h in range(4):
            o = bN[h // 2].ap[:, (h % 2) * 129:(h % 2) * 129 + 128]
            self.act(h_.ap[:, h, :], o, AF.Identity, [bN[h // 2], st], [h_], scale=st.ap[:, 4 + h:5 + h])

    def M3a(c):
        st = st3[c % 6]
        h_ = h32[c % 6]
        self.red(DVE, st.ap[:, 0:4], h_.ap[:], ALU.add, [h_], [st])
        self.tt(POOL, sq[c % 2].ap[:], h_.ap[:], h_.ap[:], ALU.mult, [h_], [sq[c % 2]])

    def M3b(c):
        st = st3[c % 6]
        self.red(DVE, st.ap[:, 4:8], sq[c % 2].ap[:], ALU.add, [sq[c % 2]], [st])
        self.ts(DVE, st.ap[:, 0:4], st.ap[:, 0:4], 1.0 / 128, None, ALU.mult, None, [st], [st])
        self.tt(DVE, st.ap[:, 8:12], st.ap[:, 0:4], st.ap[:, 0:4], ALU.mult, [st], [st])
        self.stt(DVE, st.ap[:, 4:8], st.ap[:, 4:8], 1.0 / 128, st.ap[:, 8:12], ALU.mult, ALU.subtract, [st], [st])

    def M3c(c):
        st = st3[c % 6]
        self.act(st.ap[:, 4:8], st.ap[:, 4:8], AF.Ln, [st], [st], bias=self.eps_col.ap[:, 0:1])
        self.act(st.ap[:, 4:8], st.ap[:, 4:8], AF.Exp, [st], [st], scale=-0.5)

    def M3d(c):
        csl = slice(c * 128, (c + 1) * 128)
        self.ld(mo[c % 2].ap[:], moT_v[:, :, csl], [self.R['moT']], [mo[c % 2]])
        st = st3[c % 6]
        h_ = h32[c % 6]
        self.tt(DVE, h_.ap[:], h_.ap[:], st.ap[:, 0:4].unsqueeze(2).to_broadcast([128, 4, 128]), ALU.subtract,
                [h_, st], [h_])
        hb = hnb[c % 2]
        self.tt(DVE, hb.ap[:], h_.ap[:], st.ap[:, 4:8].unsqueeze(2).to_broadcast([128, 4, 128]), ALU.mult,
                [h_, st], [hb])

    def M4(c):
        csl = slice(c * 128, (c + 1) * 128)
        hb = hnb[c % 2]
        bH = self.bank_from('mH', [6])
        bHv = bH.ap[:].bitcast(BF16)
        for h in range(4):
            self.tr(bHv[:, h * 128:(h + 1) * 128], hb.ap[:, h, :], self.ident_bf.ap[:], [hb, self.ident_bf], [bH])
        yo = ymo[c % 2]
        for h in range(4):
            self.stt(DVE, yo.ap[:, h, :], bHv[:, h * 128:(h + 1) * 128], normw.ap[:, h:h + 1], mo[c % 2].ap[:, h, :],
                     ALU.mult, ALU.mult, [bH, normw, mo[c % 2]], [yo])
        self.ld(ymT_v[:, :, csl], yo.ap[:], [yo], [self.R['ymT']])

    def M01(c):
        M0(c)
        M1(c)

    self.pipeline_fine(NT, [M01, M2, M3a, M3b, M3c, M3d, M4])
    self.pop()


Prog.phase_mlstm = _phase_mlstm


def _bank_from(self, key, lst):
    cnt = self.__dict__.setdefault('_bank_cnt', {})
    i = cnt.get(key, 0)
    cnt[key] = i + 1
    return self.banks[lst[i % len(lst)]]


Prog.bank_from = _bank_from


def _pipeline(self, n, stages):
    ns = len(stages)
    for step in range(n + ns - 1):
        for si, stg in enumerate(stages):
            k = step - si
            if 0 <= k < n:
                stg(k)


Prog.pipeline = _pipeline


def _phase_fox(self, l):
    self.push()
    fw, nc = self.fw, self.nc
    DVE, ACT, POOL, PE = self.DVE, self.ACT, self.POOL, self.PE
    self.push()
    Ga = fw.sb("f_Ga", [8, S], F32)
    ON = fw.sb("f_ON", [8, S], F32)
    Fc = fw.sb("f_F", [8, S], F32)
    FH = fw.sb("f_FH", [8, S], BF16)
    FL = fw.sb("f_FL", [8, S], BF16)
    self.ld(Ga.ap[:], self.scr['gsm'][8:16, :], [self.R['gsm']], [Ga])
    self.act(Ga.ap[:], Ga.ap[:], AF.Exp, [Ga], [Ga], scale=-1.0)
    self.act(Ga.ap[:], Ga.ap[:], AF.Ln, [Ga], [Ga], bias=1.0)
    self.memset(DVE, ON.ap[:], 1.0, [ON])
    fw.op(DVE, lambda: nc.vector.tensor_tensor_scan(out=Fc.ap[:], data0=ON.ap[:], data1=Ga.ap[:], initial=0.0,
                                                    op0=ALU.mult, op1=ALU.subtract), [ON, Ga], [Fc])
    self.cp(DVE, FH.ap[:], Fc.ap[:], [Fc], [FH])
    self.tt(DVE, FL.ap[:], Fc.ap[:], FH.ap[:], ALU.subtract, [Fc, FH], [FL])
    self.ld(self.scr['fx'][0], FH.ap[:], [FH], [self.R['fx']])
    self.ld(self.scr['fx'][1], FL.ap[:], [FL], [self.R['fx']])
    NH = fw.sb("f_NH", [8, S], BF16)
    NL = fw.sb("f_NL", [8, S], BF16)
    self.ts(DVE, NH.ap[:], FH.ap[:], -1.0, None, ALU.mult, None, [FH], [NH])
    self.ts(DVE, NL.ap[:], FL.ap[:], -1.0, None, ALU.mult, None, [FL], [NL])
    self.ld(self.scr['fx'][2], NH.ap[:], [NH], [self.R['fx']])
    self.ld(self.scr['fx'][3], NL.ap[:], [NL], [self.R['fx']])
    self.pop()
    maskneg = fw.sb("f_mask", [128, 128], F32)
    self.ld(maskneg.ap[:], self.cst['c_maskneg'], [self.R_c], [maskneg])
    Vall = fw.sb("f_V", [128, NT, 8, 65], BF16)
    for h in range(8):
        self.ld(Vall.ap[:, :, h, 0:64], self.scr['va'][:, h * 64:(h + 1) * 64].rearrange("(nt p) c -> p nt c", p=128),
                [self.R['va']], [Vall])
    self.memset(POOL, Vall.ap[:, :, :, 64:65], 1.0, [Vall])
    Ytok = fw.sb("f_Y", [128, NT, 512], BF16)
    qa = [fw.sb("f_qa%d" % i, [68, S], BF16) for i in range(2)]
    ka = [fw.sb("f_ka%d" % i, [68, S], BF16) for i in range(2)]
    for i in range(2):
        self.memset(DVE, qa[i].ap[64:68, :], 1.0, [qa[i]])
        self.memset(DVE, ka[i].ap[64:68, :], 1.0, [ka[i]])
    Ssb = [fw.sb("f_S%d" % i, [128, S], F32) for i in range(4)]
    Pbf = [fw.sb("f_P%d" % i, [128, S], BF16) for i in range(3)]
    PT = [fw.sb("f_PT%d" % i, [128, 512], BF16) for i in range(4)]
    st = fw.sb("f_st", [128, 16], F32)
    fx = self.scr['fx']
    iters = [(h, qb) for h in range(8) for qb in range(NT)]
    state = {'pti': 0}

    def load_head(h):
        q_, k_ = qa[h % 2], ka[h % 2]
        self.ld(q_.ap[0:64, :], self.scr['aqT'][h * 64:(h + 1) * 64, :], [self.R['aqT']], [q_])
        self.ld(q_.ap[66:67, :], fx[0, h:h + 1, :], [self.R['fx']], [q_])
        self.ld(q_.ap[67:68, :], fx[1, h:h + 1, :], [self.R['fx']], [q_])
        self.ld(k_.ap[0:64, :], self.scr['akT'][h * 64:(h + 1) * 64, :], [self.R['akT']], [k_])
        self.ld(k_.ap[64:65, :], fx[2, h:h + 1, :], [self.R['fx']], [k_])
        self.ld(k_.ap[65:66, :], fx[3, h:h + 1, :], [self.R['fx']], [k_])

    def stA(k):
        h, qb = iters[k]
        if qb == 0:
            if h == 0:
                load_head(0)
            if h + 1 < 8:
                load_head(h + 1)
        q_, k_ = qa[h % 2], ka[h % 2]
        qsl = slice(qb * 128, (qb + 1) * 128)
        nk = (qb + 1) * 128
        S_ = Ssb[k % 4]
        for j in range((nk + 511) // 512):
            w = min(512, nk - j * 512)
            b = self.bank_from('fS', [0, 1, 2, 3])
            self.mm(b.ap[:, 0:w], q_.ap[0:68, qsl], k_.ap[0:68, j * 512:j * 512 + w], True, True, [q_, k_], [b])
            self.cp(ACT, S_.ap[:, j * 512:j * 512 + w], b.ap[:, 0:w], [b], [S_])

    def stB(k):
        h, qb = iters[k]
        qsl = slice(qb * 128, (qb + 1) * 128)
        nk = (qb + 1) * 128
        S_ = Ssb[k % 4]
        c0 = (k % 4) * 4
        self.tt(DVE, S_.ap[:, qsl], S_.ap[:, qsl], maskneg.ap[:], ALU.add, [S_, maskneg], [S_])
        self.red(DVE, st.ap[:, c0:c0 + 1], S_.ap[:, 0:nk], ALU.max, [S_], [st])
        self.ts(DVE, st.ap[:, c0 + 1:c0 + 2], st.ap[:, c0:c0 + 1], -1.0, None, ALU.mult, None, [st], [st])

    def stC(k):
        h, qb = iters[k]
        nk = (qb + 1) * 128
        c0 = (k % 4) * 4
        self.act(Pbf[k % 3].ap[:, 0:nk], Ssb[k % 4].ap[:, 0:nk], AF.Exp, [Ssb[k % 4], st], [Pbf[k % 3]],
                 bias=st.ap[:, c0 + 1:c0 + 2])

    def stD(k):
        h, qb = iters[k]
        P_ = Pbf[k % 3]
        c0 = (k % 4) * 4
        bO = self.bank_from('fO', [6, 7])
        groups = [list(range(g * 4, min(g * 4 + 4, qb + 1))) for g in range((qb + 4) // 4)]
        pts = []

        def do_tr(kbs):
            bT = self.bank_from('fT', [4, 5])
            bTv = bT.ap[:].bitcast(BF16)
            for i, kb in enumerate(kbs):
                self.tr(bTv[:, i * 128:(i + 1) * 128], P_.ap[:, kb * 128:(kb + 1) * 128], self.ident_bf.ap[:],
                        [P_, self.ident_bf], [bT])
            pt = PT[state['pti'] % 4]
            state['pti'] += 1
            self.cp(DVE, pt.ap[:, 0:len(kbs) * 128], bTv[:, 0:len(kbs) * 128], [bT], [pt])
            return pt

        def do_pv(kbs, pt):
            for i, kb in enumerate(kbs):
                self.mm(bO.ap[:, 0:65], pt.ap[:, i * 128:(i + 1) * 128], Vall.ap[:, kb, h, :], kb == 0, kb == qb,
                        [pt, Vall], [bO])
        prev = None
        for kbs in groups:
            pt = do_tr(kbs)
            if prev is not None:
                do_pv(*prev)
            prev = (kbs, pt)
        do_pv(*prev)
        fw.op(DVE, lambda: nc.vector.reciprocal(out=st.ap[:, c0 + 2:c0 + 3], in_=bO.ap[:, 64:65]), [bO], [st])
        self.ts(DVE, Ytok.ap[:, qb, h * 64:(h + 1) * 64], bO.ap[:, 0:64], st.ap[:, c0 + 2:c0 + 3], None, ALU.mult, None,
                [bO, st], [Ytok])

    self.pipeline(len(iters), [stA, stB, stC, stD])
    yo = [fw.sb("f_yo%d" % i, [128, 4, 128], BF16) for i in range(2)]
    yaT_v = self.scr['yaT'].rearrange("(f p) t -> p f t", p=128)
    for qb in range(NT):
        bT = self.bank_from('fT', [4, 5])
        bTv = bT.ap[:].bitcast(BF16)
        for f in range(4):
            self.tr(bTv[:, f * 128:(f + 1) * 128], Ytok.ap[:, qb, f * 128:(f + 1) * 128], self.ident_bf.ap[:],
                    [Ytok, self.ident_bf], [bT])
        y_ = yo[qb % 2]
        self.cp(ACT, y_.ap[:].rearrange("p f t -> p (f t)"), bTv[:, 0:512], [bT], [y_])
        self.ld(yaT_v[:, :, qb * 128:(qb + 1) * 128], y_.ap[:], [y_], [self.R['yaT']])
    self.pop()


Prog.phase_fox = _phase_fox


def _phase_s5(self, l):
    self.push()
    fw, nc = self.fw, self.nc
    DVE, ACT, POOL, PE = self.DVE, self.ACT, self.POOL, self.PE
    inp = self.inp
    L1 = fw.sb("s_L1", [128, 32, 128], BF16)
    L2 = fw.sb("s_L2", [128, 32, 128], BF16)
    W1 = fw.sb("s_W1", [128, 512], BF16)
    W2 = fw.sb("s_W2", [128, 512], BF16)
    Dsel = fw.sb("s_Dsel", [128, 32, 16], BF16)
    thp = fw.sb("s_thp", [128, 32], F32)
    RP = fw.sb("s_RP", [128, 32], F32)
    thb = fw.sb("s_thb", [128, 32], F32)
    self.push()
    LR = fw.sb("s_LR", [128, 32], F32)
    LI = fw.sb("s_LI", [128, 32], F32)
    DT = fw.sb("s_DT", [128, 32], F32)
    for half in range(2):
        ps = slice(half * 64, half * 64 + 64)
        self.ld(LR.ap[ps, :], inp['s5_lam_re'][l].rearrange("g p -> p g"), [self.R_w], [LR], allow_slow_non_contiguous=True)
        self.ld(LI.ap[ps, :], inp['s5_lam_im'][l].rearrange("g p -> p g"), [self.R_w], [LI], allow_slow_non_contiguous=True)
    self.ld(DT.ap[:], inp['s5_log_dt'][l].partition_broadcast(128), [self.R_w], [DT])
    self.act(DT.ap[:], DT.ap[:], AF.Exp, [DT], [DT])
    t = [fw.sb("s_t%d" % i, [128, 32], F32) for i in range(12)]
    ti = fw.sb("s_ti", [128, 32], I32)
    TH, ST1, CT1, AR, AI, DEN, ZR, ZI, ZIs, ZRs2, tmpa, tmpb = t
    self.tt(DVE, TH.ap[:], LI.ap[:], DT.ap[:], ALU.mult, [LI, DT], [TH])
    self.ts(DVE, thp.ap[:], TH.ap[:], 1.0 / TWO_PI, None, ALU.mult, None, [TH], [thp])
    self.ts(DVE, thb.ap[:], thp.ap[:], 2048.0, None, ALU.mult, None, [thp], [thb])
    self.tt(DVE, tmpa.ap[:], LR.ap[:], DT.ap[:], ALU.mult, [LR, DT], [tmpa])
    self.act(RP.ap[:], tmpa.ap[:], AF.Exp, [tmpa], [RP])
    self.cp(DVE, ti.ap[:], thp.ap[:], [thp], [ti])
    self.tt(DVE, tmpa.ap[:], thp.ap[:], ti.ap[:], ALU.subtract, [thp, ti], [tmpa])
    self.act(ST1.ap[:], tmpa.ap[:], AF.Sin, [tmpa], [ST1], scale=TWO_PI)
    self.ts(DVE, tmpb.ap[:], thp.ap[:], 0.25, None, ALU.add, None, [thp], [tmpb])
    self.cp(DVE, ti.ap[:], tmpb.ap[:], [tmpb], [ti])
    self.tt(DVE, tmpb.ap[:], tmpb.ap[:], ti.ap[:], ALU.subtract, [tmpb, ti], [tmpb])
    self.act(CT1.ap[:], tmpb.ap[:], AF.Sin, [tmpb], [CT1], scale=TWO_PI)
    self.tt(DVE, AR.ap[:], RP.ap[:], CT1.ap[:], ALU.mult, [RP, CT1], [AR])
    self.tt(DVE, AI.ap[:], RP.ap[:], ST1.ap[:], ALU.mult, [RP, ST1], [AI])
    self.ts(DVE, AR.ap[:], AR.ap[:], -1.0, None, ALU.add, None, [AR], [AR])
    self.tt(DVE, DEN.ap[:], LR.ap[:], LR.ap[:], ALU.mult, [LR], [DEN])
    self.tt(DVE, tmpa.ap[:], LI.ap[:], LI.ap[:], ALU.mult, [LI], [tmpa])
    self.tt(DVE, DEN.ap[:], DEN.ap[:], tmpa.ap[:], ALU.add, [DEN, tmpa], [DEN])
    fw.op(DVE, lambda: nc.vector.reciprocal(out=DEN.ap[:], in_=DEN.ap[:]), [DEN], [DEN])
    self.tt(DVE, tmpa.ap[:], AR.ap[:], LR.ap[:], ALU.mult, [AR, LR], [tmpa])
    self.tt(DVE, tmpb.ap[:], AI.ap[:], LI.ap[:], ALU.mult, [AI, LI], [tmpb])
    self.tt(DVE, tmpa.ap[:], tmpa.ap[:], tmpb.ap[:], ALU.add, [tmpa, tmpb], [tmpa])
    self.tt(DVE, ZR.ap[:], tmpa.ap[:], DEN.ap[:], ALU.mult, [tmpa, DEN], [ZR])
    self.tt(DVE, tmpa.ap[:], AI.ap[:], LR.ap[:], ALU.mult, [AI, LR], [tmpa])
    self.tt(DVE, tmpb.ap[:], AR.ap[:], LI.ap[:], ALU.mult, [AR, LI], [tmpb])
    self.tt(DVE, tmpa.ap[:], tmpa.ap[:], tmpb.ap[:], ALU.subtract, [tmpa, tmpb], [tmpa])
    self.tt(DVE, ZI.ap[:], tmpa.ap[:], DEN.ap[:], ALU.mult, [tmpa, DEN], [ZI])
    self.cp(DVE, ZIs.ap[:], ZI.ap[:], [ZI], [ZIs])
    self.ts(DVE, ZIs.ap[0:64, :], ZI.ap[0:64, :], -1.0, None, ALU.mult, None, [ZI], [ZIs])
    self.cp(DVE, ZRs2.ap[:], ZR.ap[:], [ZR], [ZRs2])
    self.ts(DVE, ZRs2.ap[64:128, :], ZR.ap[64:128, :], -1.0, None, ALU.mult, None, [ZR], [ZRs2])
    Bst = fw.sb("s_Bst", [128, 32, 16], F32)
    Bsw = fw.sb("s_Bsw", [128, 32, 16], F32)
    bre = inp['s5_b_re'][l].rearrange("g p c -> p g c")
    bim = inp['s5_b_im'][l].rearrange("g p c -> p g c")
    self.ld(Bst.ap[0:64], bre, [self.R_w], [Bst])
    self.ld(Bst.ap[64:128], bim, [self.R_w], [Bst])
    self.ld(Bsw.ap[0:64], bim, [self.R_w], [Bsw])
    self.ld(Bsw.ap[64:128], bre, [self.R_w], [Bsw])
    M1 = fw.sb("s_M1", [128, 32, 16], F32)
    M2 = fw.sb("s_M2", [128, 32, 16], F32)
    Mt = fw.sb("s_Mt", [128, 32, 16], F32)

    def bc(z):
        return z.ap[:].unsqueeze(2).to_broadcast([128, 32, 16])
    self.tt(DVE, M1.ap[:], Bst.ap[:], bc(ZR), ALU.mult, [Bst, ZR], [M1])
    self.tt(DVE, Mt.ap[:], Bsw.ap[:], bc(ZIs), ALU.mult, [Bsw, ZIs], [Mt])
    self.tt(DVE, M1.ap[:], M1.ap[:], Mt.ap[:], ALU.add, [M1, Mt], [M1])
    self.tt(DVE, M2.ap[:], Bsw.ap[:], bc(ZRs2), ALU.mult, [Bsw, ZRs2], [M2])
    self.tt(DVE, Mt.ap[:], Bst.ap[:], bc(ZI), ALU.mult, [Bst, ZI], [Mt])
    self.tt(DVE, M2.ap[:], M2.ap[:], Mt.ap[:], ALU.add, [M2, Mt], [M2])
    Mpad = fw.sb("s_Mpad", [128, 32, 128], F32)
    for (M, Lx) in ((M1, L1), (M2, L2)):
        self.memset(POOL, Mpad.ap[:], 0.0, [Mpad])
        for gl in range(8):
            self.cp(DVE, Mpad.ap[:].rearrange("m (ft gl) k -> m ft gl k", gl=8)[:, :, gl, gl * 16:(gl + 1) * 16],
                    M.ap[:].rearrange("m (ft gl) c -> m ft gl c", gl=8)[:, :, gl, :], [M], [Mpad])
        for g4 in range(8):
            b = self.bank()
            for i in range(4):
                g = g4 * 4 + i
                self.tr(b.ap[:, i * 128:(i + 1) * 128], Mpad.ap[:, g, :], self.ident_f.ap[:], [Mpad, self.ident_f], [b])
            self.cp(ACT, Lx.ap[:, g4 * 4:(g4 + 1) * 4, :].rearrange("k g m -> k (g m)"), b.ap[:], [b], [Lx])
    CC = fw.sb("s_CC", [128, 4, 128], F32)
    CC2 = fw.sb("s_CC2", [128, 4, 128], F32)
    cre = inp['s5_c_re'][l].rearrange("(ft gl) c p -> (gl c) ft p", gl=8)
    cim = inp['s5_c_im'][l].rearrange("(ft gl) c p -> (gl c) ft p", gl=8)
    self.ld(CC.ap[:, :, 0:64], cre, [self.R_w], [CC])
    self.ld(CC.ap[:, :, 64:128], cim, [self.R_w], [CC])
    self.ld(CC2.ap[:, :, 0:64], cim, [self.R_w], [CC2])
    self.ld(CC2.ap[:, :, 64:128], cre, [self.R_w], [CC2])
    b = self.bank()
    for ft in range(4):
        self.tr(b.ap[:, ft * 128:(ft + 1) * 128], CC.ap[:, ft, :], self.ident_f.ap[:], [CC, self.ident_f], [b])
    self.cp(DVE, W1.ap[0:64, :], b.ap[0:64, :], [b], [W1])
    self.ts(DVE, W1.ap[64:128, :], b.ap[64:128, :], -1.0, None, ALU.mult, None, [b], [W1])
    b = self.bank()
    for ft in range(4):
        self.tr(b.ap[:, ft * 128:(ft + 1) * 128], CC2.ap[:, ft, :], self.ident_f.ap[:], [CC2, self.ident_f], [b])
    self.ts(DVE, W2.ap[:], b.ap[:], -1.0, None, ALU.mult, None, [b], [W2])
    esel = fw.sb("s_esel", [128, 8, 16], F32)
    dcol = fw.sb("s_dcol", [128, 4], F32)
    self.ld(esel.ap[:], self.cst['c_esel'], [self.R_c], [esel])
    self.ld(dcol.ap[:], inp['s5_d'][l].rearrange("(ft gl) c -> (gl c) ft", gl=8), [self.R_w], [dcol],
            allow_slow_non_contiguous=True)
    for ft in range(4):
        self.ts(DVE, Dsel.ap[:, ft * 8:(ft + 1) * 8, :], esel.ap[:], dcol.ap[:, ft:ft + 1], None, ALU.mult, None,
                [esel, dcol], [Dsel])
    self.pop()
    self.push()
    HS = S // 2
    iota = fw.sb("s_iota", [128, HS], F32)
    self.ld(iota.ap[:], self.cst['c_iota'][:, 0:HS], [self.R_c], [iota])
    zcol = fw.sb("s_zcol", [128, 1], F32)
    self.memset(DVE, zcol.ap[:], 0.0, [zcol])
    hpi = fw.sb("s_hpi", [128, 1], F32)
    self.memset(DVE, hpi.ap[:], TWO_PI / 4.0, [hpi])
    PH = [fw.sb("s_PH%d" % i, [128, HS], F32) for i in range(3)]
    PI = [fw.sb("s_PI%d" % i, [128, HS], I32) for i in range(2)]
    SN = [fw.sb("s_SN%d" % i, [128, HS], F32) for i in range(3)]
    CSt = [fw.sb("s_CS%d" % i, [128, HS], F32) for i in range(3)]
    XP = [fw.sb("s_XP%d" % i, [128, HS], F32) for i in range(2)]
    Gt = [fw.sb("s_G%d" % i, [128, HS], F32) for i in range(2)]
    Gc = [fw.sb("s_Gc%d" % i, [128, HS], BF16) for i in range(2)]
    Gs = [fw.sb("s_Gs%d" % i, [128, HS], BF16) for i in range(2)]
    uT = [fw.sb("s_uT%d" % i, [128, S], BF16) for i in range(2)]
    ysb = [fw.sb("s_ysb%d" % i, [16, HS], F32) for i in range(2)]
    tmp = [fw.sb("s_tmp%d" % i, [128, 512], F32) for i in range(2)]
    cnt = {'t': 0}

    def S0a(k):
        g, hf = k // 2, k % 2
        ft = g // 8
        if g % 8 == 0 and hf == 0:
            self.ld(uT[ft % 2].ap[:], self.scr['suT'][ft * 128:(ft + 1) * 128, :], [self.R['suT']], [uT[ft % 2]])
        ph, pi = PH[k % 3], PI[k % 2]
        bias = thb.ap[:, g:g + 1] if hf else zcol.ap[:, 0:1]
        self.act(ph.ap[:], iota.ap[:], AF.Identity, [iota, thp, thb, zcol], [ph], bias=bias, scale=thp.ap[:, g:g + 1])
        self.act(pi.ap[:], iota.ap[:], AF.Identity, [iota, thp, thb, zcol], [pi], bias=bias, scale=thp.ap[:, g:g + 1])

    def S0b(k):
        ph, pi = PH[k % 3], PI[k % 2]
        self.tt(DVE, ph.ap[:], ph.ap[:], pi.ap[:], ALU.subtract, [ph, pi], [ph])

    def S0c(k):
        ph = PH[k % 3]
        self.act(SN[k % 3].ap[:], ph.ap[:], AF.Sin, [ph], [SN[k % 3]], scale=TWO_PI)
        self.act(ph.ap[:], ph.ap[:], AF.Abs, [ph], [ph])
        self.act(CSt[k % 3].ap[:], ph.ap[:], AF.Sin, [ph, hpi], [CSt[k % 3]], scale=-TWO_PI, bias=hpi.ap[:, 0:1])

    def S1(k):
        g, hf = k // 2, k % 2
        u_ = uT[(g // 8) % 2]
        sn, cs, xp = SN[k % 3], CSt[k % 3], XP[k % 2]
        for j in range(4):
            sl = slice(j * 512, (j + 1) * 512)
            usl = slice(hf * HS + j * 512, hf * HS + (j + 1) * 512)
            b1 = self.bank_from('sX', [0, 1, 2, 3])
            self.mm(b1.ap[:], L1.ap[:, g, :], u_.ap[:, usl], True, True, [L1, u_], [b1])
            b2 = self.bank_from('sX', [0, 1, 2, 3])
            self.mm(b2.ap[:], L2.ap[:, g, :], u_.ap[:, usl], True, True, [L2, u_], [b2])
            tm = tmp[cnt['t'] % 2]
            cnt['t'] += 1
            self.tt(DVE, xp.ap[:, sl], cs.ap[:, sl], b1.ap[:], ALU.mult, [cs, b1], [xp])
            self.tt(DVE, tm.ap[:], sn.ap[:, sl], b2.ap[:], ALU.mult, [sn, b2], [tm])
            self.tt(POOL, xp.ap[:, sl], xp.ap[:, sl], tm.ap[:], ALU.add, [xp, tm], [xp])

    def S2(k):
        g, hf = k // 2, k % 2
        sn, cs, xp, g_ = SN[k % 3], CSt[k % 3], XP[k % 2], Gt[k % 2]
        if hf == 0:
            fw.op(DVE, lambda: nc.vector.tensor_tensor_scan(out=g_.ap[:], data0=RP.ap[:, g:g + 1].to_broadcast([128, HS]),
                                                            data1=xp.ap[:], initial=0.0, op0=ALU.mult, op1=ALU.add),
                  [RP, xp], [g_])
        else:
            gp = Gt[(k - 1) % 2]
            fw.op(DVE, lambda: nc.vector.tensor_tensor_scan(out=g_.ap[:], data0=RP.ap[:, g:g + 1].to_broadcast([128, HS]),
                                                            data1=xp.ap[:], initial=gp.ap[:, HS - 1:HS],
                                                            op0=ALU.mult, op1=ALU.add),
                  [RP, xp, gp], [g_])
        self.tt(DVE, Gc[k % 2].ap[:], cs.ap[:], g_.ap[:], ALU.mult, [cs, g_], [Gc[k % 2]])
        self.tt(POOL, Gs[k % 2].ap[:], sn.ap[:], g_.ap[:], ALU.mult, [sn, g_], [Gs[k % 2]])

    def S3(k):
        g, hf = k // 2, k % 2
        u_ = uT[(g // 8) % 2]
        y_ = ysb[k % 2]
        for j in range(4):
            sl = slice(j * 512, (j + 1) * 512)
            usl = slice(hf * HS + j * 512, hf * HS + (j + 1) * 512)
            b = self.bank_from('sY', [4, 5, 6, 7])
            self.mm(b.ap[0:16, :], W1.ap[:, g * 16:(g + 1) * 16], Gc[k % 2].ap[:, sl], True, False, [W1, Gc[k % 2]], [b])
            self.mm(b.ap[0:16, :], W2.ap[:, g * 16:(g + 1) * 16], Gs[k % 2].ap[:, sl], False, False, [W2, Gs[k % 2]], [b])
            self.mm(b.ap[0:16, :], Dsel.ap[:, g, :], u_.ap[:, usl], False, True, [Dsel, u_], [b])
            self.cp(ACT, y_.ap[:, sl], b.ap[0:16, :], [b], [y_])
        self.ld(self.scr['s5y'][g * 16:(g + 1) * 16, hf * HS:(hf + 1) * HS], y_.ap[:], [y_], [self.R['s5y']])

    self.pipeline_fine(64, [S0a, S0b, S0c, S1, S2, S3])
    self.pop()
    self.push()
    wgb = fw.sb("g_wgb", [128, 4, 512], BF16)
    self.ld(wgb.ap[:], inp['s5_w_glu'][l].rearrange("(kt p) c -> p kt c", p=128), [self.R_w], [wgb], q=POOL)
    bg = fw.sb("g_bg", [128, 4], F32)
    self.ld(bg.ap[:], inp['s5_b_glu'][l].rearrange("(t p) -> p t", p=128), [self.R_w], [bg], allow_slow_non_contiguous=True)
    xs = [fw.sb("g_xs%d" % i, [128, 4, 512], F32) for i in range(3)]
    x2 = [fw.sb("g_x2%d" % i, [128, 4, 512], F32) for i in range(2)]
    ys = [fw.sb("g_ys%d" % i, [128, 4, 512], F32) for i in range(2)]
    yb = [fw.sb("g_yb%d" % i, [128, 4, 512], BF16) for i in range(2)]
    sg = [fw.sb("g_sg%d" % i, [128, 512], F32) for i in range(2)]
    yo = [fw.sb("g_yo%d" % i, [128, 4, 512], BF16) for i in range(2)]
    s5v = self.scr['s5y'].rearrange("(f p) t -> p f t", p=128)
    ysv = self.scr['ysT'].rearrange("(f p) t -> p f t", p=128)
    GC = 0.7978845608028654

    def GA(tc):
        sl = slice(tc * 512, (tc + 1) * 512)
        self.ld(xs[tc % 3].ap[:], s5v[:, :, sl], [self.R['s5y']], [xs[tc % 3]])

    def GB(tc):
        x_, x2_, y_, yb_ = xs[tc % 3], x2[tc % 2], ys[tc % 2], yb[tc % 2]
        self.tt(POOL, x2_.ap[:], x_.ap[:], x_.ap[:], ALU.mult, [x_], [x2_])
        self.ts(DVE, x2_.ap[:], x2_.ap[:], 0.044715, 1.0, ALU.mult, ALU.add, [x2_], [x2_])
        self.tt(DVE, x2_.ap[:], x2_.ap[:], x_.ap[:], ALU.mult, [x2_, x_], [x2_])
        self.act(x2_.ap[:], x2_.ap[:], AF.Sigmoid, [x2_], [x2_], scale=2.0 * GC)
        self.tt(DVE, y_.ap[:], x_.ap[:], x2_.ap[:], ALU.mult, [x_, x2_], [y_])
        self.cp(POOL, yb_.ap[:], y_.ap[:], [y_], [yb_])

    def GD(tc):
        sl = slice(tc * 512, (tc + 1) * 512)
        y_, yb_, yo_ = ys[tc % 2], yb[tc % 2], yo[tc % 2]
        for f in range(4):
            b = self.bank()
            for kt in range(4):
                self.mm(b.ap[:], wgb.ap[:, kt, f * 128:(f + 1) * 128], yb_.ap[:, kt, :], kt == 0, kt == 3, [wgb, yb_], [b])
            sg_ = sg[f % 2]
            self.act(sg_.ap[:], b.ap[:], AF.Sigmoid, [b, bg], [sg_], bias=bg.ap[:, f:f + 1])
            self.tt(DVE, yo_.ap[:, f, :], y_.ap[:, f, :], sg_.ap[:], ALU.mult, [y_, sg_], [yo_])
        self.ld(ysv[:, :, sl], yo_.ap[:], [yo_], [self.R['ysT']])

    self.pipeline_fine(NCH, [GA, GB, GD])
    self.pop()
    self.pop()


Prog.phase_s5 = _phase_s5


def _ln_stats(self, r, st, c0, junk):
    self.act(junk.ap[:], r.ap[:], AF.Identity, [r], [junk, st], accum=st.ap[:, c0 + 0:c0 + 1])
    self.act(junk.ap[:], r.ap[:], AF.Square, [r], [junk, st], accum=st.ap[:, c0 + 1:c0 + 2])


def _ln_rstd(self, st, c0):
    DVE = self.DVE
    self.ts(DVE, st.ap[:, c0 + 2:c0 + 3], st.ap[:, c0 + 0:c0 + 1], 1.0 / D, None, ALU.mult, None, [st], [st])
    self.tt(DVE, st.ap[:, c0 + 3:c0 + 4], st.ap[:, c0 + 2:c0 + 3], st.ap[:, c0 + 2:c0 + 3], ALU.mult, [st], [st])
    self.stt(DVE, st.ap[:, c0 + 4:c0 + 5], st.ap[:, c0 + 1:c0 + 2], 1.0 / D, st.ap[:, c0 + 3:c0 + 4], ALU.mult, ALU.subtract,
             [st], [st])
    self.act(st.ap[:, c0 + 4:c0 + 5], st.ap[:, c0 + 4:c0 + 5], AF.Ln, [st], [st], bias=self.eps_col.ap[:, 0:1])
    self.act(st.ap[:, c0 + 5:c0 + 6], st.ap[:, c0 + 4:c0 + 5], AF.Exp, [st], [st], scale=-0.5)


def _ln_apply(self, r, st, c0, g_bc, b_bc, xo, xb=None):
    DVE, POOL = self.DVE, self.POOL
    self.ts(DVE, r.ap[:], r.ap[:], st.ap[:, c0 + 2:c0 + 3], st.ap[:, c0 + 5:c0 + 6], ALU.subtract, ALU.mult, [r, st], [r])
    self.tt(DVE, r.ap[:], r.ap[:], g_bc.ap[:], ALU.mult, [r, g_bc], [r])
    self.tt(DVE, xo.ap[:], r.ap[:], b_bc.ap[:], ALU.add, [r, b_bc], [xo])
    if xb is not None:
        self.cp(self.ACT, xb.ap[:], xo.ap[:], [xo], [xb])


Prog.ln_stats = _ln_stats
Prog.ln_rstd = _ln_rstd
Prog.ln_apply = _ln_apply


def _layer_norm(self, r, g_bc, b_bc, st, junk, xo, xb=None):
    self.ln_stats(r, st, 0, junk)
    self.ln_rstd(st, 0)
    self.ln_apply(r, st, 0, g_bc, b_bc, xo, xb)


def _pipeline_steps(self, n, stages):
    ns = len(stages)
    steps = []
    for step in range(n + ns - 1):
        def f(step=step):
            for si, stg in enumerate(stages):
                k = step - si
                if 0 <= k < n:
                    stg(k)
        steps.append(f)
    return steps


def _interleave(self, *lists):
    m = max(len(x) for x in lists)
    for i in range(m):
        for x in lists:
            if i < len(x):
                x[i]()


Prog.pipeline_steps = _pipeline_steps


def _pipeline_fine(self, n, stages):
    ns = len(stages)
    fw = self.fw
    for step in range(n + ns - 1):
        lists = []
        for si, stg in enumerate(stages):
            k = step - si
            if 0 <= k < n:
                fw._defer = []
                stg(k)
                lists.append(fw._defer)
                fw._defer = None
        m = max(len(x) for x in lists)
        for i in range(m):
            for x in lists:
                if i < len(x):
                    f, a, kw = x[i]
                    f(*a, **kw)


Prog.pipeline_fine = _pipeline_fine
Prog.interleave = _interleave


Prog.layer_norm = _layer_norm


def _phase_merge(self, l):
    self.push()
    fw, nc = self.fw, self.nc
    DVE, ACT, POOL, PE = self.DVE, self.ACT, self.POOL, self.PE
    inp = self.inp
    wbr = fw.sb("g_wbr", [128, 3, 4, 1024], BF16)
    wout = fw.sb("g_wout", [128, 8, 1024], BF16)
    for b in range(3):
        self.ld(wbr.ap[:, b], inp['w_branch'][l, b].rearrange("(kt p) c -> p kt c", p=128), [self.R_w], [wbr], q=POOL)
    for hf in range(2):
        self.ld(wout.ap[:, hf * 4:(hf + 1) * 4, :],
                inp['w_out'][l, hf * 512:(hf + 1) * 512, :].rearrange("(kt p) c -> p kt c", p=128),
                [self.R_w], [wout], q=POOL)
    g_bc = fw.sb("g_gbc", [128, D], F32)
    b_bc = fw.sb("g_bbc", [128, D], F32)
    self.ld(g_bc.ap[:], inp['ln1_g'][l].partition_broadcast(128), [self.R_w], [g_bc])
    self.ld(b_bc.ap[:], inp['ln1_b'][l].partition_broadcast(128), [self.R_w], [b_bc])
    yt = [[fw.sb("g_y%d_%d" % (b, i), [128, 4, 512], BF16) for b in range(3)] for i in range(2)]
    mixT = [fw.sb("g_mix%d" % i, [128, 8, 512], BF16) for i in range(2)]
    gt = [fw.sb("g_gt%d" % i, [128, 3, 512], F32) for i in range(3)]
    mt = [[fw.sb("g_mt%d_%d" % (b, i), [128, 512], F32) for b in range(3)] for i in range(2)]
    xt = [fw.sb("g_xt%d" % i, [128, D], F32) for i in range(2)]
    rt = [fw.sb("g_rt%d" % i, [128, D], F32) for i in range(4)]
    xo = [fw.sb("g_xo%d" % i, [128, D], F32) for i in range(2)]
    xb = [fw.sb("g_xb%d" % i, [128, D], BF16) for i in range(2)]
    stg = [fw.sb("g_stg%d" % i, [128, D], BF16) for i in range(2)]
    junk = fw.sb("g_junk", [128, D], F32)
    st = fw.sb("g_st", [128, 32], F32)
    ysrc = [self.scr[n].rearrange("(kt p) t -> p kt t", p=128) for n in ('ymT', 'ysT', 'yaT')]
    yres = [self.R[n] for n in ('ymT', 'ysT', 'yaT')]
    gv = self.scr['gT'].rearrange("(b f p) t -> p b f t", b=3, p=128)
    pbs = {}

    def br_steps(tc):
        sl = slice(tc * 512, (tc + 1) * 512)
        ys_ = yt[tc % 2]
        mx = mixT[tc % 2]

        def G0(ft):
            if ft == 0:
                for b in range(3):
                    self.ld(ys_[b].ap[:], ysrc[b][:, :, sl], [yres[b]], [ys_[b]])
            gi = tc * 8 + ft
            g_ = gt[gi % 3]
            self.ld(g_.ap[:], gv[:, :, ft, sl], [self.R['gT']], [g_])
            for b in range(3):
                pb = self.bank_from('mB', [0, 1, 2, 3, 4, 5])
                pbs[(gi, b)] = pb
                for kt in range(4):
                    self.mm(pb.ap[:], wbr.ap[:, b, kt, ft * 128:(ft + 1) * 128], ys_[b].ap[:, kt, :], kt == 0, kt == 3,
                            [wbr, ys_[b]], [pb])

        def G1(ft):
            gi = tc * 8 + ft
            g_ = gt[gi % 3]
            for b in range(3):
                pb = pbs.pop((gi, b))
                self.tt(DVE, mt[gi % 2][b].ap[:], pb.ap[:], g_.ap[:, b, :], ALU.mult, [pb, g_], [mt[gi % 2][b]])

        def G2(ft):
            gi = tc * 8 + ft
            m_ = mt[gi % 2]
            self.tt(DVE, m_[0].ap[:], m_[0].ap[:], m_[1].ap[:], ALU.add, [m_[0], m_[1]], [m_[0]])
            self.tt(DVE, mx.ap[:, ft, :], m_[0].ap[:], m_[2].ap[:], ALU.add, [m_[0], m_[2]], [mx])
        return self.pipeline_steps(8, [G0, G1, G2])

    def ol_steps(tc):
        mx = mixT[tc % 2]

        def T0(tq):
            i = tc * 4 + tq
            x_, r_ = xt[i % 2], rt[i % 4]
            self.ld(x_.ap[:], self.scr['xres0'][i * 128:(i + 1) * 128, :], [self.R['xres0']], [x_])
            for hf in range(2):
                pb = self.bank_from('mO', [6, 7])
                for kt in range(8):
                    self.mm(pb.ap[:], mx.ap[:, kt, tq * 128:(tq + 1) * 128], wout.ap[:, kt, hf * 512:(hf + 1) * 512],
                            kt == 0, kt == 7, [mx, wout], [pb])
                self.stt(DVE, r_.ap[:, hf * 512:(hf + 1) * 512], x_.ap[:, hf * 512:(hf + 1) * 512], ALPHA, pb.ap[:],
                         ALU.mult, ALU.add, [x_, pb], [r_])

        def T1(tq):
            i = tc * 4 + tq
            self.ln_stats(rt[i % 4], st, (i % 4) * 8, junk)

        def T2(tq):
            i = tc * 4 + tq
            self.ln_rstd(st, (i % 4) * 8)

        def T3(tq):
            i = tc * 4 + tq
            self.ln_apply(rt[i % 4], st, (i % 4) * 8, g_bc, b_bc, xo[i % 2], xb[i % 2])

        def T4(tq):
            i = tc * 4 + tq
            self.ld(self.scr['xres1'][i * 128:(i + 1) * 128, :], xo[i % 2].ap[:], [xo[i % 2]], [self.R['xres1']], q=POOL)
            self.ld(self.scr['x1b'][i * 128:(i + 1) * 128, :], xb[i % 2].ap[:], [xb[i % 2]], [self.R['x1b']], q=POOL)
            self.transpose_store(xb[i % 2].ap, xb[i % 2], self.scr['x1T'], self.R['x1T'], i, stg, i, pool=('mO', [6, 7]), q=POOL)
        return self.pipeline_steps(4, [T0, T1, T2, T3, T4])

    self.interleave(br_steps(0))
    for tc in range(NCH):
        nxt = br_steps(tc + 1) if tc + 1 < NCH else []
        self.interleave(nxt, ol_steps(tc))
    self.pop()


Prog.phase_merge = _phase_merge


def _phase_route(self, l):
    fw, nc = self.fw, self.nc
    DVE, ACT, POOL, PE = self.DVE, self.ACT, self.POOL, self.PE
    inp = self.inp
    DSTi, WT = self.DSTi, self.WT
    self.push()
    wr = fw.sb("r_wr", [128, 8, 36], BF16)
    self.ld(wr.ap[:, :, 0:4], inp['w_route_group'][l].rearrange("(kt p) c -> p kt c", p=128), [self.R_w], [wr], q=POOL)
    self.ld(wr.ap[:, :, 4:36], inp['w_route_expert'][l].rearrange("(kt p) c -> p kt c", p=128), [self.R_w], [wr], q=POOL)
    brt = fw.sb("r_brt", [128, 36], F32)
    self.ld(brt.ap[:, 0:4], inp['b_route_group'][l].partition_broadcast(128), [self.R_w], [brt])
    self.ld(brt.ap[:, 4:36], inp['b_route_expert'][l].partition_broadcast(128), [self.R_w], [brt])
    ustr = fw.sb("r_ustr", [128, 128], BF16)
    self.ld(ustr.ap[:], self.cst['c_ustrict'], [self.R_c], [ustr])
    ecap = fw.sb("r_ecap", [128, NE], F32)
    self.ld(ecap.ap[:], self.cst['c_ecap'], [self.R_c], [ecap])
    Asum = fw.sb("r_Asum", [128, NE], F32)
    Asb = fw.sb("r_Asb", [128, NE], BF16)
    self.memset(DVE, Asum.ap[:], 0.0, [Asum])
    self.memset(DVE, Asb.ap[:], 0.0, [Asb])
    xT_ = [fw.sb("r_xT%d" % i, [128, 8, 128], BF16) for i in range(2)]
    xb_ = [fw.sb("r_xb%d" % i, [128, D], BF16) for i in range(3)]
    lg_ = [fw.sb("r_lg%d" % i, [128, 36], F32) for i in range(2)]
    w_ = [fw.sb("r_w%d" % i, [128, 32], F32) for i in range(2)]
    w2_ = [fw.sb("r_w2%d" % i, [128, 8], F32) for i in range(2)]
    top8_ = [fw.sb("r_top8%d" % i, [128, 8], F32) for i in range(2)]
    A1_ = [fw.sb("r_A1%d" % i, [128, NE], F32) for i in range(2)]
    A2_ = [fw.sb("r_A2%d" % i, [128, NE], F32) for i in range(2)]
    A_ = [fw.sb("r_A%d" % i, [128, NE], F32) for i in range(2)]
    Ab_ = [fw.sb("r_Ab%d" % i, [128, NE], BF16) for i in range(2)]
    tm1 = fw.sb("r_tm1", [128, NE], F32)
    tm = fw.sb("r_tm", [128, NE], F32)
    td = fw.sb("r_td", [128, NE], F32)
    okm = fw.sb("r_okm", [128, NE], F32)
    x1Tv = self.scr['x1T'].rearrange("(kt p) t -> p kt t", p=128)

    def R0(i):
        xt, xb, lg = xT_[i % 2], xb_[i % 3], lg_[i % 2]
        self.ld(xt.ap[:], x1Tv[:, :, i * 128:(i + 1) * 128], [self.R['x1T']], [xt])
        self.ld(xb.ap[:], self.scr['x1b'][i * 128:(i + 1) * 128, :], [self.R['x1b']], [xb])
        pb = self.bank_from('rL', [0, 1])
        for kt in range(8):
            self.mm(pb.ap[:, 0:36], xt.ap[:, kt, :], wr.ap[:, kt, :], kt == 0, kt == 7, [xt, wr], [pb])
        self.tt(DVE, lg.ap[:], pb.ap[:, 0:36], brt.ap[:], ALU.add, [pb, brt], [lg])

    def R1(i):
        lg, w, top8 = lg_[i % 2], w_[i % 2], top8_[i % 2]
        A1, A2, A, Ab = A1_[i % 2], A2_[i % 2], A_[i % 2], Ab_[i % 2]
        self.red(DVE, w.ap[:, 0:1], lg.ap[:, 0:4], ALU.max, [lg], [w])
        self.ts(DVE, w.ap[:, 1:2], w.ap[:, 0:1], -1.0, None, ALU.mult, None, [w], [w])
        self.ts(DVE, w.ap[:, 4:8], lg.ap[:, 0:4], w.ap[:, 0:1], None, ALU.is_equal, None, [lg, w], [w])
        self.act(w.ap[:, 8:12], lg.ap[:, 0:4], AF.Exp, [lg, w], [w], bias=w.ap[:, 1:2], accum=w.ap[:, 2:3])
        fw.op(DVE, lambda: nc.vector.reciprocal(out=w.ap[:, 3:4], in_=w.ap[:, 2:3]), [w], [w])
        self.ts(DVE, w.ap[:, 12:16], w.ap[:, 4:8], -1.0, 1.0e9, ALU.add, ALU.mult, [w], [w])
        self.tt(DVE, tm1.ap[:].rearrange("p (g e) -> p g e", g=4), lg.ap[:, 4:36].rearrange("p (g e) -> p g e", g=4),
                w.ap[:, 12:16].unsqueeze(2).to_broadcast([128, 4, 8]), ALU.add, [lg, w], [tm1])
        fw.op(DVE, lambda: nc.vector.max(out=top8.ap[:], in_=tm1.ap[:]), [tm1], [top8])
        self.ts(DVE, A1.ap[:], tm1.ap[:], top8.ap[:, 0:1], None, ALU.is_equal, None, [tm1, top8], [A1])
        self.ts(DVE, A2.ap[:], tm1.ap[:], top8.ap[:, 1:2], None, ALU.is_equal, None, [tm1, top8], [A2])
        self.tt(DVE, A.ap[:], A1.ap[:], A2.ap[:], ALU.add, [A1, A2], [A])
        self.cp(DVE, Ab.ap[:], A.ap[:], [A], [Ab])
        self.ts(DVE, w.ap[:, 16:17], top8.ap[:, 0:1], -1.0, None, ALU.mult, None, [top8], [w])
        self.act(w.ap[:, 17:18], top8.ap[:, 1:2], AF.Exp, [top8, w], [w], bias=w.ap[:, 16:17])
        self.ts(DVE, w.ap[:, 18:19], w.ap[:, 17:18], 1.0, None, ALU.add, None, [w], [w])
        fw.op(DVE, lambda: nc.vector.reciprocal(out=w.ap[:, 19:20], in_=w.ap[:, 18:19]), [w], [w])
        self.tt(DVE, WT.ap[:, i, 0:1], w.ap[:, 19:20], w.ap[:, 3:4], ALU.mult, [w], [WT])
        self.tt(DVE, WT.ap[:, i, 1:2], WT.ap[:, i, 0:1], w.ap[:, 17:18], ALU.mult, [w, WT], [WT])

    def R2(i):
        xb, w2 = xb_[i % 3], w2_[i % 2]
        A1, A2, A, Ab = A1_[i % 2], A2_[i % 2], A_[i % 2], Ab_[i % 2]
        pp = self.bank_from('rP', [2, 3])
        self.mm(pp.ap[:, 0:NE], ustr.ap[:], Ab.ap[:], True, False, [ustr, Ab], [pp])
        self.mm(pp.ap[:, 0:NE], self.ones_bf.ap[:], Asb.ap[:], False, True, [self.ones_bf, Asb], [pp])
        self.stt(DVE, td.ap[:], pp.ap[:, 0:NE], float(CAP - 1), ecap.ap[:], ALU.min, ALU.add, [pp, ecap], [td])
        self.ts(DVE, okm.ap[:], pp.ap[:, 0:NE], float(CAP), None, ALU.is_lt, None, [pp], [okm])
        self.tt(DVE, tm.ap[:], A1.ap[:], okm.ap[:], ALU.mult, [A1, okm], [tm])
        self.red(DVE, w2.ap[:, 2:3], tm.ap[:], ALU.add, [tm], [w2])
        self.tt(DVE, tm.ap[:], A2.ap[:], okm.ap[:], ALU.mult, [A2, okm], [tm])
        self.red(DVE, w2.ap[:, 3:4], tm.ap[:], ALU.add, [tm], [w2])
        self.tt(DVE, WT.ap[:, i, :], WT.ap[:, i, :], w2.ap[:, 2:4], ALU.mult, [WT, w2], [WT])
        self.tt(DVE, tm.ap[:], A1.ap[:], td.ap[:], ALU.mult, [A1, td], [tm])
        self.red(DVE, w2.ap[:, 0:1], tm.ap[:], ALU.add, [tm], [w2])
        self.tt(DVE, tm.ap[:], A2.ap[:], td.ap[:], ALU.mult, [A2, td], [tm])
        self.red(DVE, w2.ap[:, 1:2], tm.ap[:], ALU.add, [tm], [w2])
        self.cp(DVE, DSTi.ap[:, i, :], w2.ap[:, 0:2], [w2], [DSTi])
        self.tt(DVE, Asum.ap[:], Asum.ap[:], A.ap[:], ALU.add, [Asum, A], [Asum])
        self.cp(DVE, Asb.ap[:], Asum.ap[:], [Asum], [Asb])
        for j in range(2):
            fw.dma(POOL, None, None, reads=[xb, DSTi], writes=[self.R['xdisp']],
                   fn=lambda j=j: nc.gpsimd.indirect_dma_start(
                       out=self.scr['xdisp'], out_offset=bass.IndirectOffsetOnAxis(ap=DSTi.ap[:, i, j:j + 1], axis=0),
                       in_=xb.ap[:], in_offset=None))

    self.pipeline_fine(NT, [R0, R1, R2])
    self.pop()


Prog.phase_route = _phase_route


def _phase_moe(self, l, parts=("route", "experts", "combine"), last=False):
    self.push()
    fw = self.fw
    self.DSTi = fw.sb("moe_DSTi", [128, NT, 2], I32)
    self.WT = fw.sb("moe_WT", [128, NT, 2], F32)
    if "route" in parts:
        self.phase_route(l)
        if "rinfo" in self.debug:
            dbg = fw.sb("moe_dbg", [128, NT, 4], F32)
            self.cp(self.DVE, dbg.ap[:, :, 0:2], self.DSTi.ap[:], [self.DSTi], [dbg])
            self.cp(self.DVE, dbg.ap[:, :, 2:4], self.WT.ap[:], [self.WT], [dbg])
            self.ld(self.scr['rinfo'], dbg.ap[:].rearrange("p a b -> p (a b)"), [dbg], [self.R['rinfo']])
    if "experts" in parts:
        self.phase_experts(l)
    if "combine" in parts:
        self.phase_combine(l, last)
    self.pop()


Prog.phase_moe = _phase_moe


def _phase_experts(self, l):
    self.push()
    fw, nc = self.fw, self.nc
    DVE, ACT, POOL, PE = self.DVE, self.ACT, self.POOL, self.PE
    inp = self.inp
    NB = 4
    wg = [fw.sb("e_wg%d" % i, [128, 8, 512], BF16) for i in range(NB)]
    wu = [fw.sb("e_wu%d" % i, [128, 8, 512], BF16) for i in range(NB)]
    wd = [fw.sb("e_wd%d" % i, [128, 4, 1024], BF16) for i in range(NB)]
    xr = [fw.sb("e_xr%d" % i, [128, NTB, D], BF16) for i in range(2)]
    xg = [fw.sb("e_xg%d" % i, [128, 8, CAP], BF16) for i in range(2)]
    hT = [fw.sb("e_hT%d" % i, [128, 4, CAP], BF16) for i in range(2)]
    sg = [fw.sb("e_sg%d" % i, [128, CAP], F32) for i in range(2)]
    yo = [fw.sb("e_yo%d" % i, [128, D], F32) for i in range(2)]
    cnt = {'y': 0}

    def E0(e):
        self.ld(xr[e % 2].ap[:], self.scr['xdisp'][e * CS:e * CS + CAP, :].rearrange("(b p) d -> p b d", p=128),
                [self.R['xdisp']], [xr[e % 2]])
        self.ld(wg[e % NB].ap[:], inp['moe_w_gate'][l, e].rearrange("(kt p) c -> p kt c", p=128), [self.R_w], [wg[e % NB]], q=POOL)
        self.ld(wu[e % NB].ap[:], inp['moe_w_up'][l, e].rearrange("(kt p) c -> p kt c", p=128), [self.R_w], [wu[e % NB]], q=POOL)
        self.ld(wd[e % NB].ap[:], inp['moe_w_down'][l, e].rearrange("(kt p) c -> p kt c", p=128), [self.R_w], [wd[e % NB]], q=POOL)

    def E1(e):
        xr_, xg_ = xr[e % 2], xg[e % 2]
        for kt in range(8):
            bT = self.bank_from('eT', [0, 1])
            bTv = bT.ap[:].bitcast(BF16)
            for tb in range(NTB):
                self.tr(bTv[:, tb * 128:(tb + 1) * 128], xr_.ap[:, tb, kt * 128:(kt + 1) * 128], self.ident_bf.ap[:],
                        [xr_, self.ident_bf], [bT])
            self.cp(ACT if kt % 2 else DVE, xg_.ap[:, kt, :], bTv[:, 0:CAP], [bT], [xg_])

    def E2(e):
        xg_, h_ = xg[e % 2], hT[e % 2]
        wg_, wu_ = wg[e % NB], wu[e % NB]
        for ht in range(4):
            bg = self.bank_from('eG', [2, 3, 4, 5])
            for kt in range(8):
                self.mm(bg.ap[:, 0:CAP], wg_.ap[:, kt, ht * 128:(ht + 1) * 128], xg_.ap[:, kt, :], kt == 0, kt == 7, [wg_, xg_], [bg])
            bu = self.bank_from('eG', [2, 3, 4, 5])
            for kt in range(8):
                self.mm(bu.ap[:, 0:CAP], wu_.ap[:, kt, ht * 128:(ht + 1) * 128], xg_.ap[:, kt, :], kt == 0, kt == 7, [wu_, xg_], [bu])
            s_ = sg[ht % 2]
            self.act(s_.ap[:], bg.ap[:, 0:CAP], AF.Silu, [bg], [s_])
            self.tt(DVE, h_.ap[:, ht, :], s_.ap[:], bu.ap[:, 0:CAP], ALU.mult, [s_, bu], [h_])

    def E3(e):
        h_, wd_ = hT[e % 2], wd[e % NB]
        for tb in range(NTB):
            y_ = yo[cnt['y'] % 2]
            cnt['y'] += 1
            for hf in range(2):
                bd = self.bank_from('eD', [6, 7])
                for kt in range(4):
                    self.mm(bd.ap[:], h_.ap[:, kt, tb * 128:(tb + 1) * 128], wd_.ap[:, kt, hf * 512:(hf + 1) * 512],
                            kt == 0, kt == 3, [h_, wd_], [bd])
                self.cp(ACT, y_.ap[:, hf * 512:(hf + 1) * 512], bd.ap[:], [bd], [y_])
            r0 = e * CS + tb * 128
            self.ld(self.scr['ydisp'][r0:r0 + 128, :], y_.ap[:], [y_], [self.R['ydisp']])

    self.pipeline(NE, [E0, E1, E2, E3])
    self.pop()


Prog.phase_experts = _phase_experts


def _phase_combine(self, l, last):
    self.push()
    fw, nc = self.fw, self.nc
    DVE, ACT, POOL, PE = self.DVE, self.ACT, self.POOL, self.PE
    inp = self.inp
    DSTi, WT = self.DSTi, self.WT
    g_bc = fw.sb("c_gbc", [128, D], F32)
    b_bc = fw.sb("c_bbc", [128, D], F32)
    self.ld(g_bc.ap[:], inp['ln2_g'][l].partition_broadcast(128), [self.R_w], [g_bc])
    self.ld(b_bc.ap[:], inp['ln2_b'][l].partition_broadcast(128), [self.R_w], [b_bc])
    xt = [fw.sb("c_xt%d" % i, [128, D], F32) for i in range(3)]
    y1 = [fw.sb("c_y1%d" % i, [128, D], F32) for i in range(3)]
    y2 = [fw.sb("c_y2%d" % i, [128, D], F32) for i in range(3)]
    rt = [fw.sb("c_rt%d" % i, [128, D], F32) for i in range(4)]
    xo = [fw.sb("c_xo%d" % i, [128, D], F32) for i in range(2)]
    xb = [fw.sb("c_xb%d" % i, [128, D], BF16) for i in range(2)]
    stg = [fw.sb("c_stg%d" % i, [128, D], BF16) for i in range(2)]
    junk = fw.sb("c_junk", [128, D], F32)
    st = fw.sb("c_st", [128, 32], F32)

    def C0(i):
        x_, a_, b_ = xt[i % 3], y1[i % 3], y2[i % 3]
        self.ld(x_.ap[:], self.scr['xres1'][i * 128:(i + 1) * 128, :], [self.R['xres1']], [x_])
        for j, y_ in ((0, a_), (1, b_)):
            fw.dma(POOL, None, None, reads=[self.R['ydisp'], DSTi], writes=[y_],
                   fn=lambda j=j, y_=y_: nc.gpsimd.indirect_dma_start(
                       out=y_.ap[:], out_offset=None, in_=self.scr['ydisp'],
                       in_offset=bass.IndirectOffsetOnAxis(ap=DSTi.ap[:, i, j:j + 1], axis=0)))

    def C1(i):
        x_, a_, b_, r_ = xt[i % 3], y1[i % 3], y2[i % 3], rt[i % 4]
        self.ts(DVE, r_.ap[:], a_.ap[:], WT.ap[:, i, 0:1], None, ALU.mult, None, [a_, WT], [r_])
        self.stt(DVE, r_.ap[:], b_.ap[:], WT.ap[:, i, 1:2], r_.ap[:], ALU.mult, ALU.add, [b_, WT, r_], [r_])
        self.stt(DVE, r_.ap[:], x_.ap[:], ALPHA, r_.ap[:], ALU.mult, ALU.add, [x_, r_], [r_])

    def C2(i):
        self.ln_stats(rt[i % 4], st, (i % 4) * 8, junk)

    def C3(i):
        self.ln_rstd(st, (i % 4) * 8)

    def C4(i):
        self.ln_apply(rt[i % 4], st, (i % 4) * 8, g_bc, b_bc, xo[i % 2], None if last else xb[i % 2])

    def C5(i):
        if last:
            self.ld(self.out[i * 128:(i + 1) * 128, :], xo[i % 2].ap[:], [xo[i % 2]], [self.R_out])
        else:
            self.ld(self.scr['xres0'][i * 128:(i + 1) * 128, :], xo[i % 2].ap[:], [xo[i % 2]], [self.R['xres0']])
            self.transpose_store(xb[i % 2].ap, xb[i % 2], self.scr['xT'], self.R['xT'], i, stg, i)

    self.pipeline_fine(NT, [C0, C1, C2, C3, C4, C5])
    self.pop()


Prog.phase_combine = _phase_combine


N_CORES = 8
FUSED = True


def build_program(L, first=True):
    P = Prog(L)
    if first:
        P.phase_t0()
    for l in range(L):
        P.phase_inproj(l)
        P.phase_mlstm(l)
        P.phase_fox(l)
        P.phase_s5(l)
        P.phase_merge(l)
        P.phase_moe(l, last=(l == L - 1))
    return P.finish()


def kernel(**inputs):
    x = np.ascontiguousarray(np.asarray(inputs['x'], dtype=np.float32))
    consts = host_consts()
    if FUSED:
        nc = build_program(DEPTH)
        in_maps = []
        for b in range(N_CORES):
            m = {'x': x[b]}
            for n in INPUT_NAMES:
                m[n] = np.ascontiguousarray(np.asarray(inputs[n], dtype=np.float32))
            m.update(consts)
            in_maps.append(m)
        res = run_bass_kernel_spmd(nc, in_maps, core_ids=list(range(N_CORES)))
        return np.stack([np.asarray(res.results[b]['out'], dtype=np.float32) for b in range(N_CORES)])
    nc = build_program(1)
    cur = x
    for l in range(DEPTH):
        w = {n: np.ascontiguousarray(np.asarray(inputs[n], dtype=np.float32)[l:l + 1]) for n in INPUT_NAMES}
        in_maps = []
        for b in range(N_CORES):
            m = {'x': np.ascontiguousarray(cur[b])}
            m.update(w)
            m.update(consts)
            in_maps.append(m)
        res = run_bass_kernel_spmd(nc, in_maps, core_ids=list(range(N_CORES)))
        cur = np.stack([np.asarray(res.results[b]['out'], dtype=np.float32) for b in range(N_CORES)])
    return cur
```
